# Optimizing a Trainium2 kernel written in Bass

```python
import jax, jax.numpy as jnp
from jax import lax
import numpy as np

D_MODEL = 1024
BATCH = 8
SEQ = 2048
DEPTH = 2
DEC_BATCH = 128
DEC_SEQ = 1
PAST_LEN = 16384
PAGE_SIZE = 128

N_EVEN = (DEPTH + 1) // 2
N_ODD = DEPTH // 2
MIX_WIDTH = D_MODEL
POOL_WIDTH = MIX_WIDTH // 2
POOL_GROUPS = 4
POOL_GC = POOL_WIDTH // POOL_GROUPS
POOL_WINDOWS = (2, 4, 8, 16)
POOL_BUF = max(POOL_WINDOWS) - 1
RWKV_WIDTH = MIX_WIDTH - POOL_WIDTH
RWKV_HEAD = 64
RWKV_HEADS = RWKV_WIDTH // RWKV_HEAD
W_RANK = 64
A_RANK = 64
G_RANK = 128
SHIFT_WIDTH = 3 * RWKV_WIDTH + W_RANK + A_RANK + G_RANK
IN_AB_WIDTH = POOL_WIDTH + SHIFT_WIDTH
LNX_EPS = 64e-5
CONV_WIDTH = 3
CONV_DIM = D_MODEL
D_FF = 2816
N_EXPERTS = 8
TOP_K = 2
E_FF = 3584
N_MEM = 256
X_HEADS = 4
X_HEAD_DIM = D_MODEL // X_HEADS
RMS_EPS = 1e-6
F32 = jnp.float32

kernel_name = 'pool_rwkv7_shortconv_moe_memxattn_step'


def rmsnorm(x, g):
    xf = x.astype(F32)
    y = xf * lax.rsqrt(jnp.mean(xf * xf, axis=-1, keepdims=True) + RMS_EPS)
    return (y * g.astype(F32)).astype(x.dtype)


def pool_mixer(u, u_prev, pos0, pool_w, pool_scale):
    B, T, _ = u.shape
    ext = jnp.concatenate([u_prev.astype(F32), u.astype(F32)], axis=1)
    csum = jnp.concatenate([jnp.zeros_like(ext[:, :1]), lax.cumsum(ext, axis=1)], axis=1)
    end = csum[:, POOL_BUF + 1:POOL_BUF + 1 + T]
    pos = pos0 + jnp.arange(T)
    means = []
    for g, w in enumerate(POOL_WINDOWS):
        lo, hi = g * POOL_GC, (g + 1) * POOL_GC
        start = csum[:, POOL_BUF + 1 - w:POOL_BUF + 1 - w + T, lo:hi]
        cnt = jnp.minimum(w, pos + 1).astype(F32)[None, :, None]
        means.append((end[..., lo:hi] - start) / cnt)
    d = jnp.concatenate(means, axis=-1) - ext[:, POOL_BUF:]
    d = d.reshape(B, T, POOL_GROUPS, POOL_GC)
    y = jnp.einsum('btgc,gcd->btgd', d, pool_w.astype(F32)).reshape(B, T, POOL_WIDTH)
    return y * pool_scale.astype(F32), ext[:, -POOL_BUF:]


def wkv_scan(S0, r, decay, k, v, kk, a):
    def step(S, inp):
        r_t, w_t, k_t, v_t, kk_t, a_t = inp
        s_kk = jnp.einsum('bhvk,bhk->bhv', S, kk_t)
        S = (S * w_t[:, :, None, :] - s_kk[..., None] * (kk_t * a_t)[:, :, None, :]
             + v_t[..., None] * k_t[:, :, None, :])
        return S, jnp.einsum('bhvk,bhk->bhv', S, r_t)
    xs = tuple(jnp.moveaxis(t, 1, 0) for t in (r, decay, k, v, kk, a))
    S, ys = lax.scan(step, S0.astype(F32), xs)
    return S, jnp.moveaxis(ys, 0, 1)


def rwkv7_mixer(p, prev_row, S0, e, P):
    B, T, _ = p.shape
    pf = p.astype(F32)
    shifted = jnp.concatenate([prev_row[:, None].astype(F32), pf[:, :-1]], axis=1)
    m = pf + (shifted - pf) * P['mu_shift'][e].astype(F32)
    C = RWKV_WIDTH
    r, k, v = m[..., :C], m[..., C:2 * C], m[..., 2 * C:3 * C]
    o = 3 * C
    dw = m[..., o:o + W_RANK]
    o += W_RANK
    da = m[..., o:o + A_RANK]
    o += A_RANK
    dg = m[..., o:o + G_RANK]
    w_log = -jax.nn.softplus(-(P['rw_w0'][e].astype(F32) + jnp.tanh(dw) @ P['rw_w2'][e].astype(F32))) - 0.5
    decay = jnp.exp(-jnp.exp(w_log))
    a = jax.nn.sigmoid(P['rw_a0'][e].astype(F32) + da @ P['rw_a2'][e].astype(F32))
    g = jax.nn.sigmoid(dg) @ P['rw_g2'][e].astype(F32)
    hs = lambda t: t.reshape(B, T, RWKV_HEADS, RWKV_HEAD)
    hp = lambda t: t.astype(F32).reshape(RWKV_HEADS, RWKV_HEAD)
    r, k, v, decay, a, g = hs(r), hs(k), hs(v), hs(decay), hs(a), hs(g)
    kk = k * hp(P['rw_kk'][e])
    kk = kk / jnp.maximum(jnp.sqrt(jnp.sum(kk * kk, axis=-1, keepdims=True)), 1e-12)
    k = k * (1.0 + (a - 1.0) * hp(P['rw_ka'][e]))
    S, y = wkv_scan(S0, r, decay, k, v, kk, a)
    mu = jnp.mean(y, axis=-1, keepdims=True)
    var = jnp.mean(jnp.square(y - mu), axis=-1, keepdims=True)
    yn = (y - mu) * lax.rsqrt(var + LNX_EPS)
    yn = yn * hp(P['rw_lnx_w'][e]) + hp(P['rw_lnx_b'][e])
    bonus = jnp.sum(r * k * P['rw_rk'][e].astype(F32), axis=-1, keepdims=True) * v
    out = ((yn + bonus) * g).reshape(B, T, C)
    return out, S, pf[:, -1]


def shortconv_mixer(xn, prev, o, P):
    T = xn.shape[1]
    h = (xn @ P['w_in_c'][o]).astype(F32)
    bg, cg, hv = h[..., :CONV_DIM], h[..., CONV_DIM:2 * CONV_DIM], h[..., 2 * CONV_DIM:]
    ext = jnp.concatenate([prev.astype(F32), cg * hv], axis=1)
    cw = P['conv_w'][o].astype(F32)
    z = sum(cw[j] * ext[:, j:j + T] for j in range(CONV_WIDTH))
    y = (bg * z).astype(xn.dtype) @ P['w_out_c'][o]
    return y, ext[:, -(CONV_WIDTH - 1):]


def mem_attention(xn, mem_k, mem_v, wq, wo):
    B, T, _ = xn.shape
    q = (xn @ wq).reshape(B, T, X_HEADS, X_HEAD_DIM)
    s = jnp.einsum('bthd,bmhd->bhtm', q, mem_k).astype(F32) * (X_HEAD_DIM ** -0.5)
    pr = jax.nn.softmax(s, axis=-1).astype(mem_v.dtype)
    out = jnp.einsum('bhtm,bmhd->bthd', pr, mem_v).reshape(B, T, D_MODEL)
    return out @ wo


def dense_swiglu(xn, e, P):
    h = jax.nn.silu(xn @ P['ffn_gate'][e]) * (xn @ P['ffn_up'][e])
    return h @ P['ffn_down'][e]


def moe_swiglu(xn, o, P):
    B, T, D = xn.shape
    x2 = xn.reshape(B * T, D)
    logits = (x2 @ P['router_w'][o]).astype(F32) + P['router_b'][o].astype(F32)
    top_v, top_i = lax.top_k(logits, TOP_K)
    gw = jax.nn.softmax(top_v, axis=-1)
    gates = jnp.sum(jax.nn.one_hot(top_i, N_EXPERTS, dtype=F32) * gw[..., None], axis=1)
    out = jnp.zeros((B * T, D), F32)
    for ex in range(N_EXPERTS):
        h = jax.nn.silu(x2 @ P['moe_gate'][o, ex]) * (x2 @ P['moe_up'][o, ex])
        out = out + gates[:, ex:ex + 1] * (h @ P['moe_down'][o, ex]).astype(F32)
    return out.astype(xn.dtype).reshape(B, T, D)


def trunk(x, pos0, mem_k, mem_v, pool_prev, shift_prev, wkv_prev, conv_prev, P):
    dt = x.dtype
    new_pool, new_shift, new_wkv, new_conv = [], [], [], []
    for i in range(DEPTH):
        xn = rmsnorm(x, P['norm_mix'][i])
        if i % 2 == 0:
            e = i // 2
            h = xn @ P['w_in_ab'][e]
            y_a, buf = pool_mixer(h[..., :POOL_WIDTH], pool_prev[e], pos0, P['pool_w'][e], P['pool_scale'][e])
            y_b, S, row = rwkv7_mixer(h[..., POOL_WIDTH:], shift_prev[e], wkv_prev[e], e, P)
            mix = jnp.concatenate([y_a, y_b], axis=-1).astype(dt) @ P['w_out_ab'][e]
            new_pool.append(buf.astype(dt))
            new_shift.append(row.astype(dt))
            new_wkv.append(S.astype(dt))
        else:
            o = i // 2
            mix, cbuf = shortconv_mixer(xn, conv_prev[o], o, P)
            new_conv.append(cbuf.astype(dt))
        x = x + mix
        x = x + mem_attention(rmsnorm(x, P['norm_xattn'][i]), mem_k[i], mem_v[i], P['w_xq'][i], P['w_xo'][i])
        xn = rmsnorm(x, P['norm_ffn'][i])
        x = x + (dense_swiglu(xn, i // 2, P) if i % 2 == 0 else moe_swiglu(xn, i // 2, P))
    y = rmsnorm(x, P['norm_final'])
    return y, jnp.stack(new_pool), jnp.stack(new_shift), jnp.stack(new_wkv), jnp.stack(new_conv)


def setup_inputs(seed: int = 0) -> dict:
    key = jax.random.key(seed)
    keys = jax.random.split(key, 64)
    counter = [0]
    def nk():
        kk = keys[counter[0]]
        counter[0] += 1
        return kk
    def nrm(shape, scale=1.0):
        return scale * jax.random.normal(nk(), shape, jnp.float32)
    def uni(shape, lo, hi):
        return jax.random.uniform(nk(), shape, jnp.float32, lo, hi)
    D = D_MODEL
    return {
        'x_prompt': nrm((BATCH, SEQ, D)),
        'x_sample': nrm((DEC_BATCH, DEC_SEQ, D)),
        'mem_prompt': nrm((BATCH, N_MEM, D)),
        'cache_mem_k': nrm((DEPTH, DEC_BATCH, N_MEM, X_HEADS, X_HEAD_DIM)),
        'cache_mem_v': nrm((DEPTH, DEC_BATCH, N_MEM, X_HEADS, X_HEAD_DIM)),
        'state_pool': nrm((N_EVEN, DEC_BATCH, POOL_BUF, POOL_WIDTH)),
        'state_shift': nrm((N_EVEN, DEC_BATCH, SHIFT_WIDTH)),
        'state_wkv': nrm((N_EVEN, DEC_BATCH, RWKV_HEADS, RWKV_HEAD, RWKV_HEAD), 0.5),
        'state_conv': nrm((N_ODD, DEC_BATCH, CONV_WIDTH - 1, CONV_DIM)),
        'norm_mix': 1.0 + nrm((DEPTH, D), 0.02),
        'norm_xattn': 1.0 + nrm((DEPTH, D), 0.02),
        'norm_mem': 1.0 + nrm((DEPTH, D), 0.02),
        'norm_ffn': 1.0 + nrm((DEPTH, D), 0.02),
        'norm_final': 1.0 + nrm((D,), 0.02),
        'w_xq': nrm((DEPTH, D, D), D ** -0.5),
        'w_xk': nrm((DEPTH, D, D), D ** -0.5),
        'w_xv': nrm((DEPTH, D, D), D ** -0.5),
        'w_xo': nrm((DEPTH, D, D), D ** -0.5),
        'w_in_ab': nrm((N_EVEN, D, IN_AB_WIDTH), D ** -0.5),
        'pool_w': nrm((N_EVEN, POOL_GROUPS, POOL_GC, POOL_GC), POOL_GC ** -0.5),
        'pool_scale': 1.0 + nrm((N_EVEN, POOL_WIDTH), 0.02),
        'mu_shift': uni((N_EVEN, SHIFT_WIDTH), 0.0, 1.0),
        'rw_w0': uni((N_EVEN, RWKV_WIDTH), -6.0, 0.0),
        'rw_w2': nrm((N_EVEN, W_RANK, RWKV_WIDTH), 0.5 * W_RANK ** -0.5),
        'rw_a0': nrm((N_EVEN, RWKV_WIDTH), 0.1),
        'rw_a2': nrm((N_EVEN, A_RANK, RWKV_WIDTH), 0.5 * A_RANK ** -0.5),
        'rw_g2': nrm((N_EVEN, G_RANK, RWKV_WIDTH), G_RANK ** -0.5),
        'rw_kk': 0.85 + nrm((N_EVEN, RWKV_WIDTH), 0.05),
        'rw_ka': 1.0 + nrm((N_EVEN, RWKV_WIDTH), 0.05),
        'rw_rk': nrm((N_EVEN, RWKV_HEADS, RWKV_HEAD), 0.1),
        'rw_lnx_w': 1.0 + nrm((N_EVEN, RWKV_WIDTH), 0.02),
        'rw_lnx_b': nrm((N_EVEN, RWKV_WIDTH), 0.02),
        'w_out_ab': nrm((N_EVEN, MIX_WIDTH, D), MIX_WIDTH ** -0.5),
        'ffn_gate': nrm((N_EVEN, D, D_FF), D ** -0.5),
        'ffn_up': nrm((N_EVEN, D, D_FF), D ** -0.5),
        'ffn_down': nrm((N_EVEN, D_FF, D), D_FF ** -0.5),
        'w_in_c': nrm((N_ODD, D, 3 * CONV_DIM), D ** -0.5),
        'conv_w': nrm((N_ODD, CONV_WIDTH, CONV_DIM), CONV_WIDTH ** -0.5),
        'w_out_c': nrm((N_ODD, CONV_DIM, D), CONV_DIM ** -0.5),
        'router_w': nrm((N_ODD, D, N_EXPERTS), D ** -0.5),
        'router_b': nrm((N_ODD, N_EXPERTS), 0.01),
        'moe_gate': nrm((N_ODD, N_EXPERTS, D, E_FF), D ** -0.5),
        'moe_up': nrm((N_ODD, N_EXPERTS, D, E_FF), D ** -0.5),
        'moe_down': nrm((N_ODD, N_EXPERTS, E_FF, D), E_FF ** -0.5),
    }


def reference(x_prompt, x_sample, mem_prompt, cache_mem_k, cache_mem_v, state_pool, state_shift, state_wkv, state_conv,
              norm_mix, norm_xattn, norm_mem, norm_ffn, norm_final, w_xq, w_xk, w_xv, w_xo,
              w_in_ab, pool_w, pool_scale, mu_shift, rw_w0, rw_w2, rw_a0, rw_a2, rw_g2, rw_kk, rw_ka, rw_rk,
              rw_lnx_w, rw_lnx_b, w_out_ab, ffn_gate, ffn_up, ffn_down, w_in_c, conv_w, w_out_c,
              router_w, router_b, moe_gate, moe_up, moe_down):
    P = dict(norm_mix=norm_mix, norm_xattn=norm_xattn, norm_ffn=norm_ffn, norm_final=norm_final,
             w_xq=w_xq, w_xo=w_xo, w_in_ab=w_in_ab, pool_w=pool_w, pool_scale=pool_scale, mu_shift=mu_shift,
             rw_w0=rw_w0, rw_w2=rw_w2, rw_a0=rw_a0, rw_a2=rw_a2, rw_g2=rw_g2, rw_kk=rw_kk, rw_ka=rw_ka,
             rw_rk=rw_rk, rw_lnx_w=rw_lnx_w, rw_lnx_b=rw_lnx_b, w_out_ab=w_out_ab, ffn_gate=ffn_gate,
             ffn_up=ffn_up, ffn_down=ffn_down, w_in_c=w_in_c, conv_w=conv_w, w_out_c=w_out_c,
             router_w=router_w, router_b=router_b, moe_gate=moe_gate, moe_up=moe_up, moe_down=moe_down)
    bp = x_prompt.shape[0]
    dt = x_prompt.dtype
    mk, mv = [], []
    for i in range(DEPTH):
        mn = rmsnorm(mem_prompt, norm_mem[i])
        mk.append((mn @ w_xk[i]).reshape(bp, N_MEM, X_HEADS, X_HEAD_DIM))
        mv.append((mn @ w_xv[i]).reshape(bp, N_MEM, X_HEADS, X_HEAD_DIM))
    mem_k_p = jnp.stack(mk)
    mem_v_p = jnp.stack(mv)
    y_prompt, pool_p, shift_p, wkv_p, conv_p = trunk(
        x_prompt, 0, mem_k_p, mem_v_p,
        jnp.zeros((N_EVEN, bp, POOL_BUF, POOL_WIDTH), dt),
        jnp.zeros((N_EVEN, bp, SHIFT_WIDTH), dt),
        jnp.zeros((N_EVEN, bp, RWKV_HEADS, RWKV_HEAD, RWKV_HEAD), dt),
        jnp.zeros((N_ODD, bp, CONV_WIDTH - 1, CONV_DIM), dt), P)
    y_sample, pool_s, shift_s, wkv_s, conv_s = trunk(
        x_sample, PAST_LEN, cache_mem_k, cache_mem_v, state_pool, state_shift, state_wkv, state_conv, P)
    return (y_prompt, y_sample, pool_p, pool_s, shift_p, shift_s, wkv_p, wkv_s, conv_p, conv_s, mem_k_p, mem_v_p)
```

```python
import numpy as np
import contextlib
import os
SKIP = os.environ.get('KSKIP', '')
import concourse.bass as bass
import concourse.mybir as mybir
from concourse.bass_utils import run_bass_kernel_spmd

F32 = mybir.dt.float32
BF16 = mybir.dt.bfloat16
AF = mybir.ActivationFunctionType
ALU = mybir.AluOpType
AX = mybir.AxisListType

NT = 2064
TT = [(0, 512), (512, 512), (1024, 512), (1536, 512), (2048, 16)]
D = 1024
NSLOT = 34
RMS_EPS = 1e-6
LNX_EPS = 64e-5
POOL_W = (2, 4, 8, 16)

VEC_COLS = {}


def _vec_layout():
    off = 0
    def add(name, n):
        nonlocal off
        VEC_COLS[name] = off
        off += n
    for nm in ("norm_mix", "norm_xattn", "norm_mem", "norm_ffn"):
        add(nm + "0", 8)
        add(nm + "1", 8)
    add("norm_final", 8)
    add("pool_scale", 4)
    add("mu_shift", 14)
    for nm in ("rw_w0", "rw_a0", "rw_kk", "rw_ka", "rw_rk", "rw_lnx_w", "rw_lnx_b"):
        add(nm, 4)
    add("conv_w0", 8)
    add("conv_w1", 8)
    add("conv_w2", 8)
    add("router_b", 1)
    add("omu", 14)
    add("omka", 4)
    return off


NVEC = _vec_layout()

C_ID = 0
C_BLK = 128
C_E = 256
C_RC = 320
C_ONE = 384
C_EPS = 512
C_IOTA = 520
NCONST = 648
B_ID = 0; B_ONE = 128; B_BM = 256; B_IE = 384; B_MSU = 576; B_MSL = 768; B_RST = 896
NCB = 1408


def _consts():
    c = np.zeros((128, NCONST), np.float32)
    cb = np.zeros((128, NCB), np.float32)
    p = np.arange(128)
    c[:, C_ID:C_ID + 128] = np.eye(128, dtype=np.float32)
    hh = p // 64
    c[:, C_BLK:C_BLK + 128] = (hh[:, None] == hh[None, :]).astype(np.float32)
    bm = np.zeros((128, 2, 64), np.float32)
    bm[p, hh, :] = 1.0
    s = p % 64
    t = np.arange(64)
    su = (t[None, :] > s[:, None]).astype(np.float32)
    ui = (t[None, :] >= s[:, None]).astype(np.float32)
    sl = (t[None, :] < s[:, None]).astype(np.float32)
    E = (s[:, None] == t[None, :]).astype(np.float32)
    c[:, C_E:C_E + 64] = E
    for g, w in enumerate(POOL_W):
        c[:, C_RC + g * 16:C_RC + (g + 1) * 16] = 1.0 / np.minimum(w, np.arange(16) + 1.0)
    c[:, C_ONE:C_ONE + 128] = 1.0
    c[:, C_EPS] = RMS_EPS
    c[:, C_EPS + 1] = LNX_EPS
    c[:, C_IOTA:C_IOTA + 128] = np.arange(128, dtype=np.float32)[None, :]
    cb[:, B_ID:B_ID + 128] = np.eye(128, dtype=np.float32)
    cb[:, B_ONE:B_ONE + 128] = 1.0
    cb[:, B_BM:B_BM + 128] = bm.reshape(128, 128)
    cb[:, B_IE:B_IE + 128] = np.eye(128, dtype=np.float32)
    cb[:, B_IE + 128:B_IE + 192] = E
    cb[:, B_MSU:B_MSU + 128] = (bm * su[:, None, :]).reshape(128, 128)
    cb[:, B_MSU + 128:B_MSU + 192] = ui
    cb[:, B_MSL:B_MSL + 128] = (bm * sl[:, None, :]).reshape(128, 128)
    rst = np.ones(512, np.float32)
    rst[::64] = 0.0
    cb[:, B_RST:B_RST + 512] = rst[None, :]
    return c, cb


def _pack_vecs(inp):
    v = np.zeros((128, NVEC), np.float32)
    def put(name, arr):
        a = np.asarray(arr, np.float32).reshape(-1)
        n = a.size // 128
        v[:, VEC_COLS[name]:VEC_COLS[name] + n] = a.reshape(n, 128).T
    for nm in ("norm_mix", "norm_xattn", "norm_mem", "norm_ffn"):
        put(nm + "0", inp[nm][0])
        put(nm + "1", inp[nm][1])
    put("norm_final", inp["norm_final"])
    put("pool_scale", inp["pool_scale"][0])
    put("mu_shift", inp["mu_shift"][0])
    for nm in ("rw_w0", "rw_a0", "rw_kk", "rw_ka", "rw_rk", "rw_lnx_w", "rw_lnx_b"):
        put(nm, inp[nm][0])
    for j in range(3):
        put("conv_w%d" % j, inp["conv_w"][0, j])
    v[0:8, VEC_COLS["router_b"]] = np.asarray(inp["router_b"], np.float32).reshape(8)
    return v


class Prog:
    def __init__(self, nc):
        self.nc = nc
        self.ops = []
        self.res = {}
        self.ndma = 28
        self.dma_last = [None] * self.ndma
        self.dma_cnt = [0] * self.ndma
        self.rr = 0
        self.cur_region = None
        self.regions = []
        self.flag_ap = None
        self.flag_key = None

    def region_begin(self, sense):
        self.regions.append(dict(sense=sense))
        self.cur_region = len(self.regions) - 1

    def region_end(self):
        self.cur_region = None

    def flagload(self, engs, keys):
        for e in engs:
            i = self.op(e, None, keys, ())
            self.ops[i]["flagload"] = True

    def op(self, eng, fn, r=(), w=(), dma=False):
        i = len(self.ops)
        deps = {}
        for k in r:
            e = self.res.get(k)
            if e is not None and e[0] is not None:
                deps[e[0]] = "raw"
        for k in w:
            e = self.res.get(k)
            if e is not None:
                if e[0] is not None:
                    deps.setdefault(e[0], "waw")
                for rd in e[1]:
                    deps.setdefault(rd, "war")
        o = dict(eng=eng, fn=fn, dma=dma, deps=deps, sig=False, val=0, region=self.cur_region)
        if dma:
            j = self.rr
            self.rr = (self.rr + 1) % self.ndma
            if self.dma_last[j] is not None:
                deps[self.dma_last[j]] = "raw"
            self.dma_cnt[j] += 16
            o["sem"] = j
            o["dval"] = self.dma_cnt[j]
            self.dma_last[j] = i
        deps.pop(i, None)
        self.ops.append(o)
        for k in r:
            self.res.setdefault(k, [None, []])[1].append(i)
        for k in w:
            self.res[k] = [i, []]
        return i

    def emit(self):
        nc = self.nc
        ops = self.ops
        engs = ["pe", "act", "dve", "pool", "sp"]
        for o in ops:
            waits = []
            for d, kind in o["deps"].items():
                do = ops[d]
                if do["dma"]:
                    waits.append(d)
                elif do["eng"] == o["eng"] and not o["dma"]:
                    if o["eng"] != "pe" and kind == "raw":
                        waits.append(d)
                else:
                    waits.append(d)
            o["waits"] = waits
        for e in engs:
            widx = {}
            saved = None
            cur = None
            for o in ops:
                if o["eng"] != e:
                    continue
                rg = o["region"]
                if rg != cur:
                    if cur is not None:
                        widx = saved
                    if rg is not None:
                        saved = dict(widx)
                    cur = rg
                kept = []
                for d in sorted(o["waits"], reverse=True):
                    do = ops[d]
                    key = ("d", do["sem"]) if do["dma"] else do["eng"]
                    if widx.get(key, -1) >= d:
                        continue
                    widx[key] = d
                    kept.append(d)
                o["waits"] = kept
                for d in kept:
                    if not ops[d]["dma"]:
                        ops[d]["sig"] = True
        cnt = {e: 0 for e in engs}
        for o in ops:
            if o["sig"]:
                cnt[o["eng"]] += 1
                o["val"] = cnt[o["eng"]]
        self.sig_counts = dict(cnt)
        self.n_ops = {e: sum(1 for o in ops if o['eng'] == e) for e in engs}
        with contextlib.ExitStack() as st:
            sems = {e: st.enter_context(nc.semaphore("s_" + e)) for e in engs}
            dsem = [st.enter_context(nc.semaphore("d%d" % j)) for j in range(self.ndma)]
            block = st.enter_context(nc.Block())
            dma_cnt = self.dma_cnt

            nreg = len(self.regions)
            comp = [{e: 0 for e in engs} for _ in range(nreg)]
            dcomp = [{e: {} for e in engs} for _ in range(nreg)]
            for o in ops:
                rg = o["region"]
                if rg is None:
                    continue
                if o["dma"]:
                    dd = dcomp[rg][o["eng"]]
                    dd[o["sem"]] = dd.get(o["sem"], 0) + 16
                elif o["sig"]:
                    comp[rg][o["eng"]] += 1
            flag_ap = self.flag_ap

            def run(ename, E):
                waited = {}
                saved = None
                cur = None
                guard = None
                reg = None
                fval = None
                involved = set(o["region"] for o in ops if o["eng"] == ename and o["region"] is not None)

                def close_region():
                    nonlocal guard, cur, waited, saved
                    guard.__exit__(None, None, None)
                    els = E.Else()
                    els.__enter__()
                    E.drain()
                    if comp[cur][ename]:
                        E.sem_inc(sems[ename], comp[cur][ename])
                    for j, v in dcomp[cur][ename].items():
                        E.sem_inc(dsem[j], v)
                    els.__exit__(None, None, None)
                    waited = saved
                    guard = None
                    cur = None

                for o in ops:
                    if o["eng"] != ename:
                        continue
                    rg = o["region"]
                    if rg != cur:
                        if cur is not None:
                            close_region()
                        if rg is not None:
                            assert reg is not None, "flag not loaded on " + ename
                            saved = dict(waited)
                            guard = E.If(fval > 0) if self.regions[rg]["sense"] else E.If(fval == 0)
                            guard.__enter__()
                            cur = rg
                    for d in o["waits"]:
                        do = ops[d]
                        if do["dma"]:
                            key, val, sm = ("d", do["sem"]), do["dval"], dsem[do["sem"]]
                        else:
                            key, val, sm = do["eng"], do["val"], sems[do["eng"]]
                        E.wait_ge(sm, val)
                    if o.get("flagload"):
                        reg = E.alloc_register("flag_" + ename)
                        E.reg_load(reg, flag_ap)
                        fval = E.snap(reg)
                        continue
                    ins = o["fn"](E)
                    if o["dma"]:
                        ins.then_inc(dsem[o["sem"]], 16)
                    elif o["sig"]:
                        ins.then_inc(sems[ename], 1)
                if cur is not None:
                    close_region()
                if ename == "sp":
                    for j in range(self.ndma):
                        if dma_cnt[j] > 0:
                            E.wait_ge(dsem[j], dma_cnt[j])

            @block.tensor
            def _(E):
                run("pe", E)

            @block.scalar
            def _(E):
                run("act", E)

            @block.vector
            def _(E):
                run("dve", E)

            @block.gpsimd
            def _(E):
                run("pool", E)

            @block.sync
            def _(E):
                run("sp", E)


def build_program(nc, dbg_stage=99):
    P = Prog(nc)
    st = contextlib.ExitStack()

    USED = []
    NEED = {"moe_gate": 6, "moe_up": 6, "moe_down": 6, "router_w": 6, "ffn_gate": 3, "ffn_up": 3, "ffn_down": 3,
            "w_in_c": 4, "w_out_c": 4, "state_conv": 4, "w_xq": 2, "w_xk": 2, "w_xv": 2, "w_xo": 2,
            "cache_mem_k": 2, "cache_mem_v": 2, "mem_prompt": 2}

    def din(name, shape):
        if dbg_stage < NEED.get(name, 0):
            return None
        USED.append(name)
        return nc.dram_tensor(name, list(shape), F32, kind="ExternalInput").ap()

    def dout(name, shape):
        return nc.dram_tensor(name, list(shape), F32, kind="ExternalOutput").ap()

    xp = din("x_prompt", [2048, D])
    xs = din("x_sample", [16, D])
    memp = din("mem_prompt", [256, D])
    ck = din("cache_mem_k", [2, 16, 256, D])
    cv = din("cache_mem_v", [2, 16, 256, D])
    spool = din("state_pool", [16, 15, 512])
    sshift = din("state_shift", [16, 1792])
    swkv = din("state_wkv", [16, 8, 64, 64])
    sconv = din("state_conv", [16, 2, D])
    w_xq = din("w_xq", [2, D, D]); w_xk = din("w_xk", [2, D, D]); w_xv = din("w_xv", [2, D, D]); w_xo = din("w_xo", [2, D, D])
    w_in_ab = din("w_in_ab", [D, 2304])
    pool_w = din("pool_w", [4, 128, 128])
    rw_w2 = din("rw_w2", [64, 512]); rw_a2 = din("rw_a2", [64, 512]); rw_g2 = din("rw_g2", [128, 512])
    w_out_ab = din("w_out_ab", [D, D])
    ffn_gate = din("ffn_gate", [D, 2816]); ffn_up = din("ffn_up", [D, 2816]); ffn_down = din("ffn_down", [2816, D])
    w_in_c = din("w_in_c", [D, 3072]); w_out_c = din("w_out_c", [D, D])
    router_w = din("router_w", [D, 8])
    moe_gate = din("moe_gate", [8, D, 3584]); moe_up = din("moe_up", [8, D, 3584]); moe_down = din("moe_down", [8, 3584, D])
    vecs_d = din("vecs", [128, NVEC])
    consts_d = din("consts", [128, NCONST])
    constsb_d = din("constsb", [128, NCB])

    y_prompt = dout("y_prompt", [2048, D]); y_sample = dout("y_sample", [16, D])
    pool_p = dout("pool_p", [15, 512]); pool_s = dout("pool_s", [16, 15, 512])
    shift_p = dout("shift_p", [14, 128]); shift_s = dout("shift_s", [16, 1792])
    wkv_p = dout("wkv_p", [8, 64, 64]); wkv_s = dout("wkv_s", [16, 8, 64, 64])
    conv_p = dout("conv_p", [2, D]); conv_s = dout("conv_s", [16, 2, D])
    mem_k_p = dout("mem_k_p", [2, 256, D]); mem_v_p = dout("mem_v_p", [2, 256, D])

    def sb(name, shape, dt=F32):
        return st.enter_context(nc.sbuf_tensor(name, list(shape), dt))

    X = sb("X", [128, 8, NT])
    XN = sb("XN", [128, 8, NT], BF16)
    A1 = sb("A1", [128, 8, NT], BF16)
    AR = sb("AR", [128, NSLOT, 512])
    CF = sb("CF", [128, NCONST])
    CB = sb("CB", [128, NCB], BF16)
    VEC = sb("VEC", [128, NVEC])
    SM = sb("SM", [128, 1024])
    FLI = sb("FLI", [1, 2], mybir.dt.int32)
    PS = st.enter_context(nc.psum_tensor("PS", [128, 8, 512], F32))

    identF = CF[:, C_ID:C_ID + 128]
    blkF = CF[:, C_BLK:C_BLK + 128]
    onesF = CF[:, C_ONE:C_ONE + 128]
    EF = CF[:, C_E:C_E + 64]
    EPSC = CF[:, C_EPS:C_EPS + 1]
    EPSL = CF[:, C_EPS + 1:C_EPS + 2]
    identB = CB[:, B_ID:B_ID + 128]
    onesB = CB[:, B_ONE:B_ONE + 128]
    bmB = CB[:, B_BM:B_BM + 128]
    IEB = CB[:, B_IE:B_IE + 192]
    mskB = CB[:, B_MSU:B_MSU + 192]
    mslB = CB[:, B_MSL:B_MSL + 128]
    RSTM = CB[:, B_RST:B_RST + 512]

    def vcol(name, j=0):
        c = VEC_COLS[name] + j
        return VEC[:, c:c + 1]

    def kX(kc, tt): return ("X", kc, tt)
    def kXN(kc, tt): return ("XN", kc, tt)
    def kA1(kc, tt): return ("A1", kc, tt)
    def kS(i, n=1): return [("AR", i + j) for j in range(n)]
    KA1ALL = [kA1(kc, ti) for kc in range(8) for ti in range(5)]
    kC = ("C",)
    kCB = ("CB",)
    kV = ("V",)
    kSM = lambda nm: ("SM", nm)
    kW = lambda nm: ("W", nm)

    def arf(i, n=1):
        return AR[:, i:i + n, :].rearrange("p a b -> p (a b)")

    def arb(i, n=1):
        return AR[:, i:i + n, :].rearrange("p a b -> p (a b)").bitcast(BF16)

    bank_rr = [0]

    def bank():
        b = bank_rr[0]
        bank_rr[0] = (b + 1) % 8
        return b

    def bank2():
        b0 = bank()
        while b0 % 2:
            b0 = bank()
        b1 = bank()
        return b0, b1

    def kP(b): return ("ps", b)

    def mm(out, lhsT, rhs, start, stop, r, w):
        return P.op("pe", lambda E: E.matmul(out, lhsT, rhs, start=start, stop=stop), r, w)

    def act(out, in_, func, r, w, scale=1.0, bias=None, accum=None):
        kw = {}
        if bias is not None:
            kw["bias"] = bias
        if accum is not None:
            kw["accum_out"] = accum
        return P.op("act", lambda E: E.activation(out, in_, func, scale=scale, **kw), r, w)

    def tt_(out, in0, in1, alu, r, w, eng="dve"):
        return P.op(eng, lambda E: E.tensor_tensor(out, in0, in1, alu), r, w)

    def ts_(out, in0, s1, s2, op0, op1, r, w, eng="dve"):
        return P.op(eng, lambda E: E.tensor_scalar(out, in0, s1, s2, op0, op1), r, w)

    def ts1(out, in0, s1, op0, r, w, eng="dve"):
        return P.op(eng, lambda E: E.tensor_single_scalar(out, in0, s1, op0), r, w)

    def stt(out, in0, scalar, in1, op0, op1, r, w):
        return P.op("dve", lambda E: E.scalar_tensor_tensor(out, in0, scalar, in1, op0, op1), r, w)

    def cp(out, in_, r, w, eng="dve"):
        if eng == "act":
            return P.op("act", lambda E: E.copy(out, in_), r, w)
        return P.op(eng, lambda E: E.tensor_copy(out, in_), r, w)

    def red(out, in_, alu, r, w):
        return P.op("dve", lambda E: E.tensor_reduce(out, in_, AX.X, alu), r, w)

    def recip(out, in_, r, w):
        return P.op("dve", lambda E: E.reciprocal(out, in_), r, w)

    def dma(out, in_, r, w, eng="sp"):
        return P.op(eng, lambda E: E.dma_start(out=out, in_=in_), r, w, dma=True)

    def memset(ap, val, w, eng="dve"):
        return P.op(eng, lambda E: E.memset(ap, val), (), w)

    FEN = SM[:, 1016:1024]

    def fence(r, w):
        return P.op("dve", lambda E: E.memset(FEN, 0.0), list(r), list(w) + [kSM("fence")])

    dma(CF[:], consts_d[:, :], [], [kC])
    dma(VEC[:], vecs_d[:, :], [], [kV])
    dma(CB[:], constsb_d[:, :], [], [kCB], eng="pool")
    c0 = VEC_COLS["mu_shift"]
    ts_(VEC[:, VEC_COLS["omu"]:VEC_COLS["omu"] + 14], VEC[:, c0:c0 + 14], -1.0, 1.0, ALU.mult, ALU.add, [kV], [kV])
    c0 = VEC_COLS["rw_ka"]
    ts_(VEC[:, VEC_COLS["omka"]:VEC_COLS["omka"] + 4], VEC[:, c0:c0 + 4], -1.0, 1.0, ALU.mult, ALU.add, [kV], [kV])

    SM_PL, SM_HS, SM_PSS, SM_US, SM_PC = 0, 16, 272, 496, 560
    SM_CT, SM_CS, SM_SCT = 624, 640, 16
    SM_TMP = 272
    SM_T0 = 1000
    memset(SM[:, 0:272], 0.0, [kSM("PL"), kSM("HS")])
    memset(SM[:, SM_PC:SM_PC + 64], 0.0, [kSM("PC")])

    for i in range(16):
        s0 = (i % 2) * 2
        dma(arf(s0, 2), xp[i * 128:(i + 1) * 128, :], [], kS(s0, 2))
        for hf in range(2):
            b = bank()
            for j in range(4):
                kc = hf * 4 + j
                mm(PS[:, b, j * 128:(j + 1) * 128], arf(s0, 2)[:, kc * 128:(kc + 1) * 128], identF, True, True,
                   kS(s0, 2) + [kC], [kP(b)])
            cp(X[:, hf * 4:hf * 4 + 4, i * 128:(i + 1) * 128], PS[:, b, :].rearrange("p (a b) -> p a b", a=4),
               [kP(b)], [kX(hf * 4 + j, i // 4) for j in range(4)], eng=("act" if hf else "dve"))
    dma(arf(5, 2)[0:16, :], xs[:, :], [], kS(5, 2))
    b = bank()
    for kc in range(8):
        mm(PS[:, b, kc * 16:(kc + 1) * 16], arf(5, 2)[0:16, kc * 128:(kc + 1) * 128], identF[0:16, 0:16], True, True,
           kS(5, 2) + [kC], [kP(b)])
    cp(X[:, :, 2048:2064], PS[:, b, 0:128].rearrange("p (a b) -> p a b", a=8), [kP(b)], [kX(kc, 4) for kc in range(8)])

    SQ0, RSTD = 0, 4
    RSTD_L = [4]

    def rms_stats(ti, o, n):
        RSTD = RSTD_L[0]
        sq = arb(SQ0, 4).rearrange("p (a b) -> p a b", a=8)
        for kc in range(8):
            act(sq[:, kc, 0:n], X[:, kc, o:o + n], AF.Square, [kX(kc, ti)], kS(SQ0, 4))
        b = bank()
        for kc in range(8):
            mm(PS[:, b, 0:n], onesB, sq[:, kc, 0:n], kc == 0, kc == 7, kS(SQ0, 4) + [kCB], [kP(b)])
        rs = arf(RSTD)[:, 0:n]
        act(rs, PS[:, b, 0:n], AF.Ln, [kP(b), kC], kS(RSTD), scale=1.0 / D, bias=EPSC)
        act(rs, rs, AF.Exp, kS(RSTD), kS(RSTD), scale=-0.5)
        return rs

    def rmsnorm(gname, hook=None):
        for ti, (o, n) in enumerate(TT):
            rs = rms_stats(ti, o, n)
            if hook is not None:
                hook(ti, o, n, rs)
            for kc in range(8):
                stt(XN[:, kc, o:o + n], X[:, kc, o:o + n], vcol(gname, kc), rs, ALU.mult, ALU.mult,
                    [kX(kc, ti), kV] + kS(RSTD), [kXN(kc, ti)])

    def out_proj(wd):
        for hf in range(2):
            s0 = 21 + hf * 4
            wv = arb(s0, 4).rearrange("p (a b) -> p a b", a=8)
            dma(wv, wd[:, hf * 512:(hf + 1) * 512].rearrange("(kc p) o -> p kc o", p=128), [], kS(s0, 4), eng="pool")
            for ti, (o, n) in enumerate(TT):
                for j in range(4):
                    oc = hf * 4 + j
                    b = bank()
                    for kc in range(8):
                        mm(PS[:, b, 0:n], wv[:, kc, j * 128:(j + 1) * 128], A1[:, kc, o:o + n], kc == 0, kc == 7,
                           kS(s0, 4) + [kA1(kc, ti)], [kP(b)])
                    tt_(X[:, oc, o:o + n], PS[:, b, 0:n], X[:, oc, o:o + n], ALU.add, [kP(b), kX(oc, ti)], [kX(oc, ti)])

    KTs, VBs, MNs = 5, 7, 9

    def memkv(li):
        mn = arb(MNs, 2).rearrange("p (a b) -> p a b", a=8)
        kt = arb(KTs, 2).rearrange("p (a b) -> p a b", a=8)
        vb = arb(VBs, 2).rearrange("p (a b) -> p a b", a=2)
        for mb in range(2):
            s0 = 29 + mb * 2
            stg = arf(s0, 2)
            dma(stg, memp[mb * 128:(mb + 1) * 128, :], [], kS(s0, 2))
            ss = SM[:, SM_T0 + mb:SM_T0 + mb + 1]
            kss = kSM("t%d" % mb)
            tt_(arf(0, 2), stg, stg, ALU.mult, kS(s0, 2), kS(0, 2))
            red(ss, arf(0, 2), ALU.add, kS(0, 2), [kss])
            act(ss, ss, AF.Sqrt, [kss, kC], [kss], scale=1.0 / D, bias=EPSC)
            recip(ss, ss, [kss], [kss])
            ts1(stg, stg, ss, ALU.mult, kS(s0, 2) + [kss], kS(s0, 2))
            for kc in range(8):
                b = bank()
                mm(PS[:, b, 0:128], stg[:, kc * 128:(kc + 1) * 128], identF, True, True, kS(s0, 2) + [kC], [kP(b)])
                ts1(mn[:, kc, mb * 128:(mb + 1) * 128], PS[:, b, 0:128], vcol("norm_mem%d" % li, kc), ALU.mult,
                    [kP(b), kV], kS(MNs, 2))
        for which, wd, od in (((0, w_xk, mem_k_p), (1, w_xv, mem_v_p)) if 'mkproj' not in SKIP else ()):
            for hf in range(2):
                s0 = 21 + hf * 4
                wv = arb(s0, 4).rearrange("p (a b) -> p a b", a=8)
                dma(wv, wd[li, :, hf * 512:(hf + 1) * 512].rearrange("(kc p) o -> p kc o", p=128), [], kS(s0, 4), eng="pool")
                for mb in range(2):
                    b = bank()
                    for kc in range(8):
                        mm(PS[:, b, :], mn[:, kc, mb * 128:(mb + 1) * 128], wv[:, kc, :], kc == 0, kc == 7,
                           kS(MNs, 2) + kS(s0, 4), [kP(b)])
                    so = (which * 4 + hf * 2 + mb) % 4
                    cp(arf(so), PS[:, b, :], [kP(b)], kS(so), eng="act")
                    if 'mkout' not in SKIP:
                        dma(od[li, mb * 128:(mb + 1) * 128, hf * 512:(hf + 1) * 512], arf(so), kS(so), [("out", "memkv")])
                    if which == 1 and 'mkvb' not in SKIP:
                        cp(vb[:, mb, hf * 512:(hf + 1) * 512], arf(so), kS(so), kS(VBs, 2), eng="act")
                if which == 0 and 'mkkt' not in SKIP:
                    for j in range(4):
                        oc = hf * 4 + j
                        b = bank()
                        for kc in range(8):
                            mm(PS[:, b, 0:256], wv[:, kc, j * 128:(j + 1) * 128], mn[:, kc, :], kc == 0, kc == 7,
                               kS(MNs, 2) + kS(s0, 4), [kP(b)])
                        cp(kt[:, oc, :], PS[:, b, 0:256], [kP(b)], kS(KTs, 2))

    def xattn(li):
        rmsnorm("norm_xattn%d" % li)
        if 'memkv' not in SKIP:
            memkv(li)
        kt = arb(KTs, 2).rearrange("p (a b) -> p a b", a=8)
        vb = arb(VBs, 2).rearrange("p (a b) -> p a b", a=2)
        QT = 11
        qt = arb(QT, 5)[:, 0:2 * NT].rearrange("p (a b) -> p a b", a=2)
        QS = 16
        qs = arf(QS)[:, 0:128].rearrange("p (a b) -> p a b", a=8)
        EB, RI = 17, 19
        for h in range(4):
            s0 = 29 + (h % 2) * 2
            wv = arb(s0, 2).rearrange("p (a b) -> p a b", a=8)
            dma(wv, w_xq[li, :, h * 256:(h + 1) * 256].rearrange("(kc p) o -> p kc o", p=128), [], kS(s0, 2), eng="pool")
            for dc in range(2):
                for ti, (o, n) in enumerate(TT):
                    b = bank()
                    for kc in range(8):
                        mm(PS[:, b, 0:n], wv[:, kc, dc * 128:(dc + 1) * 128], XN[:, kc, o:o + n], kc == 0, kc == 7,
                           kS(s0, 2) + [kXN(kc, ti)], [kP(b)])
                    if ti < 4:
                        act(qt[:, dc, o:o + n], PS[:, b, 0:n], AF.Copy, [kP(b)], kS(QT, 5), scale=1.0 / 16.0)
                    else:
                        act(qs[:, 2 * h + dc, :], PS[:, b, 0:n], AF.Copy, [kP(b)], kS(QS), scale=1.0 / 16.0)
            for ti, (o, n) in enumerate(TT[:4] if 'attn' not in SKIP else []):
                es = EB + (ti % 2)
                eb = arb(es).rearrange("p (a b) -> p a b", a=2)
                for mc in range(2):
                    b = bank()
                    for dc in range(2):
                        mm(PS[:, b, 0:n], kt[:, 2 * h + dc, mc * 128:(mc + 1) * 128], qt[:, dc, o:o + n], dc == 0, dc == 1,
                           kS(KTs, 2) + kS(QT, 5), [kP(b)])
                    act(eb[:, mc, 0:n], PS[:, b, 0:n], AF.Exp, [kP(b)], kS(es))
                b = bank()
                for mc in range(2):
                    mm(PS[:, b, 0:n], onesB, eb[:, mc, 0:n], mc == 0, mc == 1, kS(es) + [kCB], [kP(b)])
                ri = arf(RI + (ti % 2))[:, 0:n]
                act(ri, PS[:, b, 0:n], AF.Ln, [kP(b)], kS(RI + (ti % 2)))
                act(ri, ri, AF.Exp, kS(RI + (ti % 2)), kS(RI + (ti % 2)), scale=-1.0)
                for dc in range(2):
                    b = bank()
                    for mc in range(2):
                        mm(PS[:, b, 0:n], vb[:, mc, h * 256 + dc * 128:h * 256 + (dc + 1) * 128], eb[:, mc, 0:n], mc == 0, mc == 1,
                           kS(VBs, 2) + kS(es), [kP(b)])
                    tt_(A1[:, 2 * h + dc, o:o + n], PS[:, b, 0:n], ri, ALU.mult, [kP(b)] + kS(RI + (ti % 2)), [kA1(2 * h + dc, ti)])
        for bsmp in range(16 if 'samp' not in SKIP else 0):
            par = bsmp % 2
            KBk, KBv = 21, 25
            RB = 17 if par == 0 else 11
            TP = 29 if par == 0 else 0
            kb = arf(KBk, 4).rearrange("p (a b) -> p a b", a=2)
            vv = arf(KBv, 4).rearrange("p (a b) -> p a b", a=2)
            dma(kb, ck[li, bsmp].rearrange("(j p) x -> p j x", p=128), [], kS(KBk, 4))
            dma(vv, cv[li, bsmp].rearrange("(j p) x -> p j x", p=128), [], kS(KBv, 4))
            R = arf(RB, 2).rearrange("p (a b) -> p a b", a=8)
            tt_(R, qs[:, :, bsmp:bsmp + 1].broadcast_to([128, 8, 128]), identF.unsqueeze(1).broadcast_to([128, 8, 128]),
                ALU.mult, kS(QS) + [kC], kS(RB, 2))
            b0, b1 = bank2()
            for hf, bb in ((0, b0), (1, b1)):
                mm(PS[:, bb, :], onesF, arf(RB, 2)[:, hf * 512:(hf + 1) * 512], True, True, kS(RB, 2) + [kC], [kP(bb)])
            qbc = PS[:, b0:b0 + 2, :].rearrange("p a b -> p (a b)")
            tp = arf(TP, 4)
            tt_(tp.rearrange("p (j x) -> p j x", j=2), kb, qbc.unsqueeze(1).broadcast_to([128, 2, 1024]), ALU.mult,
                kS(KBk, 4) + [kP(b0), kP(b1)], kS(TP, 4))
            so_ = SM_T0 + 2 if par == 0 else 920
            sc = SM[:, so_:so_ + 8]
            ksc, kee, ks1 = kSM("sc%d" % par), ("EE", par), kSM("s1%d" % par)
            red(sc, tp.rearrange("p (a d) -> p a d", d=256), ALU.add, kS(TP, 4), [ksc])
            ee = arf(33)[:, par * 8:par * 8 + 8]
            act(ee, sc, AF.Exp, [ksc], [kee])
            b = bank()
            mm(PS[:, b, 0:8], onesF, ee, True, True, [kee, kC], [kP(b)])
            s1 = SM[:, so_ + 8:so_ + 12]
            cp(s1, PS[:, b, 0:4], [kP(b)], [ks1])
            tt_(s1, PS[:, b, 4:8], s1, ALU.add, [kP(b), ks1], [ks1])
            recip(s1, s1, [ks1], [ks1])
            b = bank()
            for oc in range(8):
                hh_ = oc // 2
                for j in range(2):
                    mm(PS[:, b, oc:oc + 1], vv[:, j, oc * 128:(oc + 1) * 128], ee[:, j * 4 + hh_:j * 4 + hh_ + 1], j == 0, j == 1,
                       kS(KBv, 4) + [kee], [kP(b)])
            for hh_ in range(4):
                ts1(A1[:, 2 * hh_:2 * hh_ + 2, 2048 + bsmp], PS[:, b, 2 * hh_:2 * hh_ + 2], s1[:, hh_:hh_ + 1], ALU.mult,
                    [kP(b), ks1], [kA1(2 * hh_, 4), kA1(2 * hh_ + 1, 4)])
        out_proj(w_xo[li])

    A1F = A1[:].rearrange("p a b -> p (a b)")

    def ffn_run(wg, wu, wdn, FF, gate_fn=None):
        nfc = FF // 128
        groups = [(f, min(4, nfc - f)) for f in range(0, nfc, 4)]
        for gi, (f0, nf) in enumerate(groups):
            s0 = 14 + (gi % 2) * 8
            wgv = arb(s0, 4).rearrange("p (a b) -> p a b", a=8)
            wuv = arb(s0 + 4, 4).rearrange("p (a b) -> p a b", a=8)
            wdv = A1F[:, (gi % 2) * 4096:(gi % 2 + 1) * 4096].rearrange("p (a b) -> p a b", a=4)
            kwd = [("A1W", gi % 2)]
            dma(wgv[:, :, 0:nf * 128], wg[:, f0 * 128:(f0 + nf) * 128].rearrange("(kc p) f -> p kc f", p=128), [], kS(s0, 4), eng="pool")
            dma(wuv[:, :, 0:nf * 128], wu[:, f0 * 128:(f0 + nf) * 128].rearrange("(kc p) f -> p kc f", p=128), [], kS(s0 + 4, 4), eng="pool")
            dma(wdv[:, 0:nf, :], wdn[f0 * 128:(f0 + nf) * 128, :].rearrange("(fc p) o -> p fc o", p=128), [], kwd, eng="pool")
            for ti, (o, n) in enumerate(TT):
                hs = (ti % 2) * 2
                hh = arb(hs, 2).rearrange("p (a b) -> p a b", a=4)
                hkeys = kS(hs, 2)
                gb = gate_fn(ti, o, n) if gate_fn is not None else None
                for fc in range(nf):
                    bg_ = bank()
                    for kc in range(8):
                        mm(PS[:, bg_, 0:n], wgv[:, kc, fc * 128:(fc + 1) * 128], XN[:, kc, o:o + n], kc == 0, kc == 7,
                           kS(s0, 4) + [kXN(kc, ti)], [kP(bg_)])
                    bu_ = bank()
                    for kc in range(8):
                        mm(PS[:, bu_, 0:n], wuv[:, kc, fc * 128:(fc + 1) * 128], XN[:, kc, o:o + n], kc == 0, kc == 7,
                           kS(s0 + 4, 4) + [kXN(kc, ti)], [kP(bu_)])
                    sgs = 4 + (fc % 2)
                    sg = arf(sgs)[:, 0:n]
                    act(sg, PS[:, bg_, 0:n], AF.Silu, [kP(bg_)], kS(sgs))
                    if gb is None:
                        tt_(hh[:, fc, 0:n], PS[:, bu_, 0:n], sg, ALU.mult, [kP(bu_)] + kS(sgs), hkeys)
                    else:
                        tt_(sg, PS[:, bu_, 0:n], sg, ALU.mult, [kP(bu_)] + kS(sgs), kS(sgs))
                        tt_(hh[:, fc, 0:n], sg, gb[0], ALU.mult, kS(sgs) + gb[1], hkeys)
                for oc in range(8):
                    b = bank()
                    for fc in range(nf):
                        mm(PS[:, b, 0:n], wdv[:, fc, oc * 128:(oc + 1) * 128], hh[:, fc, 0:n], fc == 0, fc == nf - 1,
                           kwd + hkeys, [kP(b)])
                    tt_(X[:, oc, o:o + n], PS[:, b, 0:n], X[:, oc, o:o + n], ALU.add, [kP(b), kX(oc, ti)], [kX(oc, ti)])

    def moe_routed(POSM, GTM):
        MT = [(i * 384, 384, [3 * i, 3 * i + 1, 3 * i + 2]) for i in range(5)] + [(1920, 144, [15, 16])]
        XNF = XN[:].rearrange("p a b -> p (a b)")
        YG = XNF[:, 0:12288].bitcast(F32).rearrange("p (t o) -> p t o", t=6)
        STa = XNF[:, 12288:14592].rearrange("p (t n) -> p t n", t=6)
        HHr = XNF[:, 14592:15616].rearrange("p (b f s) -> p b f s", b=2, f=4)
        SGr = XNF[:, 15616:16128]
        XNT16 = arb(0)
        xgall = arb(2, 6).rearrange("p (k s) -> p k s", k=8)
        XGK = [("XG", t) for t in range(6)]
        hhA = arb(9).rearrange("p (f s) -> p f s", f=2)
        hhB = XNF[:, 14592:15616].rearrange("p (f s) -> p f s", f=2)

        def hhf(fc):
            return hhA[:, fc, :] if fc < 2 else hhB[:, fc - 2, :]
        BT = [(0, 512, [0, 1, 2, 3]), (512, 256, [4, 5])]
        iota = CF[:, C_IOTA:C_IOTA + 128]
        KXNALL = [kXN(kc, ti) for kc in range(8) for ti in range(5)]

        def xnt(blk):
            return A1F[:, blk * 1024:(blk + 1) * 1024] if blk < 16 else XNT16

        def xkeys(oc, o, n):
            return [kX(oc, t) for t in range(5) if TT[t][0] < o + n and TT[t][0] + TT[t][1] > o]
        fence(KA1ALL + kS(0), [("XNT", b_) for b_ in range(17)])
        memset(XNT16, 0.0, [("XNT", 16)])
        for blk in range(17):
            n = 128 if blk < 16 else 16
            o = blk * 128
            b0, b1 = bank2()
            for kc in range(8):
                bb = b0 if kc < 4 else b1
                mm(PS[0:n, bb, (kc % 4) * 128:(kc % 4 + 1) * 128], XN[:, kc, o:o + n], identB, True, True,
                   [kXN(kc, min(4, o // 512)), kCB], [kP(bb)])
            cp(xnt(blk)[0:n, 0:512], PS[0:n, b0, :], [kP(b0)], [("XNT", blk)], eng="act")
            cp(xnt(blk)[0:n, 512:1024], PS[0:n, b1, :], [kP(b1)], [("XNT", blk)])
        fence(KXNALL, [("YG", t, h) for t in range(6) for h in range(2)] + [("ST", t) for t in range(6)] + [("SGR",), ("HHR", 0), ("HHR", 1), ("HHR", 2), ("HHR", 3)] + kS(9))
        for e in range(8):
            for ti, (o, n, blks) in enumerate(MT):
                nb = len(blks)
                b0_ = blks[0]
                ssl = 1 if ti % 2 == 0 else 8
                sel = arb(ssl)[:, 0:nb * 128].rearrange("p (b s) -> p b s", b=nb)
                selg = arb(ssl)[:, 384:384 + nb * 128].rearrange("p (b s) -> p b s", b=nb)
                tt_(sel, iota.unsqueeze(1).broadcast_to([128, nb, 128]),
                    POSM[:, b0_:b0_ + nb, e:e + 1].broadcast_to([128, nb, 128]), ALU.is_equal, [kC, kSM("POSM")], [("SEL", ssl)])
                tt_(selg, sel, GTM[:, b0_:b0_ + nb, e:e + 1].broadcast_to([128, nb, 128]), ALU.mult,
                    [("SEL", ssl), kSM("GTM")], [("SELG", ssl)])
                g0, g1 = bank2()
                for kc in range(8):
                    bb = g0 if kc < 4 else g1
                    for bi, blk in enumerate(blks):
                        nt = 128 if blk < 16 else 16
                        mm(PS[:, bb, (kc % 4) * 128:(kc % 4 + 1) * 128], xnt(blk)[0:nt, kc * 128:(kc + 1) * 128], sel[0:nt, bi, :],
                           bi == 0, bi == nb - 1, [("XNT", blk), ("SEL", ssl)], [kP(bb)])
                cp(xgall[:, 0:4, ti * 128:(ti + 1) * 128], PS[:, g0, :].rearrange("p (k s) -> p k s", k=4), [kP(g0)], [("XG", ti)], eng="act")
                cp(xgall[:, 4:8, ti * 128:(ti + 1) * 128], PS[:, g1, :].rearrange("p (k s) -> p k s", k=4), [kP(g1)], [("XG", ti)])
                b = bank()
                for bi, blk in enumerate(blks):
                    nt = 128 if blk < 16 else 16
                    mm(PS[:, b, bi * 128:bi * 128 + nt], selg[0:nt, bi, :], identB[0:nt, 0:nt], True, True,
                       [("SELG", ssl), kCB], [kP(b)])
                cp(STa[:, ti, 0:n], PS[:, b, 0:n], [kP(b)], [("ST", ti)], eng="act")
            for gi in range(7):
                f0 = gi * 4
                s0 = 10 + (gi % 2) * 12
                wgv = arb(s0, 4).rearrange("p (a b) -> p a b", a=8)
                wuv = arb(s0 + 4, 4).rearrange("p (a b) -> p a b", a=8)
                wdv = arb(s0 + 8, 4).rearrange("p (a b) -> p a b", a=4)
                dma(wgv, moe_gate[e][:, f0 * 128:(f0 + 4) * 128].rearrange("(kc p) f -> p kc f", p=128), [], kS(s0, 4), eng="pool")
                dma(wuv, moe_up[e][:, f0 * 128:(f0 + 4) * 128].rearrange("(kc p) f -> p kc f", p=128), [], kS(s0 + 4, 4), eng="pool")
                dma(wdv, moe_down[e][f0 * 128:(f0 + 4) * 128, :].rearrange("(fc p) o -> p fc o", p=128), [], kS(s0 + 8, 4), eng="pool")
                for (sb0, nn, tis) in BT:
                    xk = [("XG", t) for t in tis]
                    for fc in range(4):
                        bg_, bu_ = bank(), bank()
                        for kc in range(8):
                            mm(PS[:, bg_, 0:nn], wgv[:, kc, fc * 128:(fc + 1) * 128], xgall[:, kc, sb0:sb0 + nn], kc == 0, kc == 7,
                               kS(s0, 4) + xk, [kP(bg_)])
                        for kc in range(8):
                            mm(PS[:, bu_, 0:nn], wuv[:, kc, fc * 128:(fc + 1) * 128], xgall[:, kc, sb0:sb0 + nn], kc == 0, kc == 7,
                               kS(s0 + 4, 4) + xk, [kP(bu_)])
                        act(SGr[:, 0:nn], PS[:, bg_, 0:nn], AF.Silu, [kP(bg_)], [("SGR",)])
                        tt_(hhf(fc)[:, 0:nn], PS[:, bu_, 0:nn], SGr[:, 0:nn], ALU.mult, [kP(bu_), ("SGR",)], [("HHR", fc)])
                    for j, ti in enumerate(tis):
                        for hf in range(2):
                            bd = bank()
                            for fc in range(4):
                                mm(PS[:, bd, :], hhf(fc)[:, j * 128:(j + 1) * 128], wdv[:, fc, hf * 512:(hf + 1) * 512], fc == 0, fc == 3,
                                   [("HHR", fc)] + kS(s0 + 8, 4), [kP(bd)])
                            ygk = [("YG", ti, hf)]
                            if gi == 0:
                                cp(YG[:, ti, hf * 512:(hf + 1) * 512], PS[:, bd, :], [kP(bd)], ygk, eng=("act" if hf else "dve"))
                            else:
                                tt_(YG[:, ti, hf * 512:(hf + 1) * 512], PS[:, bd, :], YG[:, ti, hf * 512:(hf + 1) * 512], ALU.add, [kP(bd)] + ygk, ygk)
            for ti, (o, n, blks) in enumerate(MT):
                ygb = arb(2 + ti)
                cp(ygb, YG[:, ti, :], [("YG", ti, 0), ("YG", ti, 1)], XGK + [("YGB", ti)], eng="act")
                for oc in range(8):
                    b = bank()
                    mm(PS[:, b, 0:n], ygb[:, oc * 128:(oc + 1) * 128], STa[:, ti, 0:n], True, True, XGK + [("YGB", ti), ("ST", ti)], [kP(b)])
                    tt_(X[:, oc, o:o + n], PS[:, b, 0:n], X[:, oc, o:o + n], ALU.add, [kP(b)] + xkeys(oc, o, n), xkeys(oc, o, n))
        fence([("XNT", b_) for b_ in range(17)], KA1ALL)

    def ffn_dense():
        rmsnorm("norm_ffn0")
        fence(KA1ALL, [("A1W", 0), ("A1W", 1)])
        ffn_run(ffn_gate, ffn_up, ffn_down, 2816)
        fence([("A1W", 0), ("A1W", 1)], KA1ALL)

    def moe():
        WR = arf(6)[:, 0:64].rearrange("p (kc e) -> p kc e", kc=8)
        dma(WR, router_w.rearrange("(kc p) e -> p kc e", p=128), [], kS(6))
        for kc in range(8):
            ts1(WR[:, kc, :], WR[:, kc, :], vcol("norm_ffn1", kc), ALU.mult, kS(6) + [kV], kS(6))
        LGT = arf(7, 5)[:, 0:NT]
        SEL = arf(12, 2).rearrange("p (e q) -> p e q", e=8)
        cp(SEL[0:8], identF[0:8, 0:8].unsqueeze(2).broadcast_to([8, 8, 128]), [kC], kS(12, 2))

        def hook(ti, o, n, rs):
            b = bank()
            for kc in range(8):
                mm(PS[0:8, b, 0:n], WR[:, kc, :], X[:, kc, o:o + n], kc == 0, kc == 7, kS(6) + [kX(kc, ti)], [kP(b)])
            tt_(LGT[0:8, o:o + n], PS[0:8, b, 0:n], rs[0:8, :], ALU.mult, [kP(b)] + kS(RSTD), kS(7, 5))
            ts1(LGT[0:8, o:o + n], LGT[0:8, o:o + n], VEC[0:8, VEC_COLS["router_b"]:VEC_COLS["router_b"] + 1], ALU.add,
                kS(7, 5) + [kV], kS(7, 5))
        rmsnorm("norm_ffn1", hook=hook)
        blocks = [(i * 128, 128) for i in range(16)] + [(2048, 16)]
        GTM = SM[:, 336:472].rearrange("p (b e) -> p b e", e=8)
        POSM = SM[:, 768:904].rearrange("p (b e) -> p b e", e=8)
        memset(SM[:, 336:472], 0.0, [kSM("GTM")])
        memset(SM[:, 768:904], -1.0, [kSM("POSM")])
        T = SM[:, SM_TMP:SM_TMP + 64]
        kt_ = [kSM("top")]
        for (o, n) in blocks:
            b = bank()
            mm(PS[0:n, b, 0:8], LGT[0:8, o:o + n], identF[0:8, 0:8], True, True, kS(7, 5) + [kC], [kP(b)])
            lg = T[0:n, 0:8]; m1 = T[0:n, 8:9]; mk1 = T[0:n, 16:24]; l2 = T[0:n, 24:32]; m2 = T[0:n, 9:10]; mk2 = T[0:n, 32:40]
            w1 = T[0:n, 10:11]; w2 = T[0:n, 11:12]; gt = T[0:n, 40:48]
            cp(lg, PS[0:n, b, 0:8], [kP(b)], kt_)
            red(m1, lg, ALU.max, kt_, kt_)
            ts1(mk1, lg, m1, ALU.is_equal, kt_, kt_)
            stt(l2, mk1, -1e30, lg, ALU.mult, ALU.add, kt_, kt_)
            red(m2, l2, ALU.max, kt_, kt_)
            ts1(mk2, l2, m2, ALU.is_equal, kt_, kt_)
            tt_(w2, m2, m1, ALU.subtract, kt_, kt_)
            act(w2, w2, AF.Exp, kt_, kt_)
            ts1(w1, w2, 1.0, ALU.add, kt_, kt_)
            recip(w1, w1, kt_, kt_)
            tt_(w2, w2, w1, ALU.mult, kt_, kt_)
            ts1(gt, mk1, w1, ALU.mult, kt_, kt_)
            stt(gt, mk2, w2, gt, ALU.mult, ALU.add, kt_, kt_)
            b = bank()
            mm(PS[0:8, b, 0:n], gt, identF[0:n, 0:n], True, True, kt_ + [kC], [kP(b)])
            cp(LGT[0:8, o:o + n], PS[0:8, b, 0:n], [kP(b)], kS(7, 5))
            cp(GTM[0:n, o // 128, :], gt, kt_, [kSM("GTM")])
        MK = arf(14, 5)[0:8, 0:NT]; POS = arf(19, 5)[0:8, 0:NT]; RS = arf(24, 5)[0:8, 0:NT]
        ts1(MK, LGT[0:8, :], 0.0, ALU.is_gt, kS(7, 5), kS(14, 5))
        memset(RS, 1.0, kS(24, 5))
        for t0_ in range(0, 2064, 384):
            memset(RS[:, t0_:t0_ + 1], 0.0, kS(24, 5))
        P.op("dve", lambda E: E.tensor_tensor_scan(POS, RS, MK, 0.0, ALU.mult, ALU.add), kS(24, 5) + kS(14, 5), kS(19, 5))
        CN = SM[0:8, SM_T0:SM_T0 + 6]
        cp(CN[:, 0:5], POS[:, 0:1920].rearrange("p (t n) -> p t n", n=384)[:, :, 383], kS(19, 5), [kSM("CN")])
        cp(CN[:, 5:6], POS[:, 2063:2064], kS(19, 5), [kSM("CN")])
        m8 = SM[0:8, SM_T0 + 6:SM_T0 + 7]
        red(m8, CN, ALU.max, [kSM("CN")], [kSM("m8")])
        b = bank()
        mm(PS[0:1, b, 0:8], m8, identF[0:8, 0:8], True, True, [kSM("m8"), kC], [kP(b)])
        mx = SM[0:1, SM_T0 + 8:SM_T0 + 9]
        red(mx, PS[0:1, b, 0:8], ALU.max, [kP(b)], [kSM("mx")])
        ts1(mx, mx, 128.5, ALU.is_gt, [kSM("mx")], [kSM("mx")])
        cp(FLI[0:1, 0:1], mx, [kSM("mx")], [("FLI",)])
        tt_(POS, POS, MK, ALU.mult, kS(19, 5) + kS(14, 5), kS(19, 5))
        ts1(POS, POS, -1.0, ALU.add, kS(19, 5), kS(19, 5))
        for (o, n) in blocks:
            b = bank()
            mm(PS[0:n, b, 0:8], POS[0:8, o:o + n], identF[0:8, 0:8], True, True, kS(19, 5) + [kC], [kP(b)])
            cp(POSM[0:n, o // 128, :], PS[0:n, b, 0:8], [kP(b)], [kSM("POSM")])
        P.flag_ap = FLI[0:1, 0:1]
        P.flagload(["pe", "act", "dve", "pool"], [("FLI",)])
        if 'noroute' in SKIP:
            P.region_begin(1)
            P.region_end()
        P.region_begin(1)
        fence(KA1ALL, [("A1W", 0), ("A1W", 1)])
        gbt = arb(30, 3)
        for e in range(8):
            def gate_fn(ti, o, n, e=e):
                return (gbt[:, ti * 512:ti * 512 + n], [("GB", ti)])
            for ti, (o, n) in enumerate(TT):
                b = bank()
                mm(PS[:, b, 0:n], SEL[0:8, e, :], LGT[0:8, o:o + n], True, True, kS(7, 5) + kS(12, 2), [kP(b)])
                cp(gbt[:, ti * 512:ti * 512 + n], PS[:, b, 0:n], [kP(b)], [("GB", ti)], eng="act")
            ffn_run(moe_gate[e], moe_up[e], moe_down[e], 3584, gate_fn=gate_fn)
        fence([("A1W", 0), ("A1W", 1)], KA1ALL)
        P.region_end()
        P.region_begin(0)
        moe_routed(POSM, GTM)
        P.region_end()

    S_R, S_K, S_V, S_A, S_G, S_LW, S_KK, S_T1, S_T2, S_BON = 0, 1, 2, 3, 28, 29, 30, 31, 32, 33
    S_Y = S_T2
    XNFL = XN[:].rearrange("p a b -> p (a b)")
    XNL = XNFL[:, 0:4096].rearrange("p (a b) -> p a b", a=8)
    RX = XNFL[:, 4096:16512]
    RA = AR[:, 4:12, :].rearrange("p a b -> p (a b)").bitcast(BF16)
    RBg = AR[:, 19:22, :].rearrange("p a b -> p (a b)").bitcast(BF16)
    NC4 = 8

    def carve(reg, off, shp, dt=BF16):
        n = shp[0] * shp[1]
        if dt == F32:
            v = reg[:, off:off + 2 * n].bitcast(F32)
        else:
            v = reg[:, off:off + n]
        return v.rearrange("p (a b) -> p a b", a=shp[0])
    WX1 = carve(RX, 0, [NC4, 320]); WBU = carve(RX, 2560, [NC4, 320]); W4 = carve(RX, 5120, [NC4, 512])
    WGT = carve(RX, 9216, [NC4, 128], F32)
    WBR = arb(14, 2)[:, 0:NC4 * 192].rearrange("p (a b) -> p a b", a=NC4)
    WNQ = W4[:, :, 0:256]
    WNT = W4[:, :, 256:512]
    WZA = carve(RA, 0, [NC4, 192]); WZK = carve(RA, 1536, [NC4, 192]); WCC = carve(RA, 3072, [NC4, 64], F32)
    WA = carve(RA, 4096, [NC4, 128]); WK = carve(RA, 5120, [NC4, 128]); WV = carve(RA, 6144, [NC4, 128]); WAp = carve(RA, 7168, [NC4, 128])
    WKp = carve(RBg, 0, [NC4, 128]); WVV = carve(RBg, 1024, [NC4, 192])
    WHP = arb(17)[:, 0:256].rearrange("p (a b) -> p a b", a=2)
    WRH = arb(17)[:, 256:768].rearrange("p (a b) -> p a b", a=NC4)
    WNAMES = ["X1", "BU", "NQ", "GT", "BR", "VV", "ZA", "ZK", "CC", "A", "K", "V", "Ap", "Kp", "NT", "HP0", "HP1", "RH"]
    WKEYS = [kW(nm) for nm in WNAMES]
    REGKEYS = kS(4, 8) + kS(19, 3) + kS(17)

    KXNALL0 = [kXN(kc, ti) for kc in range(8) for ti in range(5)]
    kXL = lambda kc: ("XNL", kc)

    def l0_mixer_run():
        fence(KXNALL0, [kXL(kc) for kc in range(8)] + WKEYS)
        PW = arb(12)[:, 0:512].rearrange("p (a b) -> p a b", a=4)
        dma(PW, pool_w.rearrange("g c d -> c g d"), [], kS(12), eng="pool")
        W2A2 = arb(13)[:, 0:512]
        dma(W2A2[0:64, :], rw_w2[:, :], [], kS(13), eng="pool")
        dma(W2A2[64:128, :], rw_a2[:, :], [], kS(13), eng="pool")
        G2 = arb(13)[:, 512:1024]
        dma(G2, rw_g2[:, :], [], kS(13), eng="pool")
        PWK = arf(14, 2)
        PC = SM[:, SM_PC:SM_PC + 64].rearrange("p (g x) -> p g x", g=4)
        PL = SM[:, SM_PL:SM_PL + 14]
        HS = SM[:, SM_HS:SM_HS + 256].rearrange("p (g v) -> p g v", g=4)
        PSS = SM[:, SM_PSS:SM_PSS + 224].rearrange("p (c b) -> p c b", c=14)
        US = SM[:, SM_US:SM_US + 64].rearrange("p (g b) -> p g b", g=4)
        TDA = arb(16)[:, 0:512]
        SGB = arb(16)[:, 512:1024]
        DPOOL = arb(27)[:, 0:512]
        SSH = arf(18)[:, 0:224].rearrange("p (c b) -> p c b", c=14)
        SPT = arf(4, 2)[:, 0:960].rearrange("p (g b r) -> p g b r", g=4, b=16)
        wp_rr = [0]

        def proj(col0, ti, o, n):
            s = 22 + wp_rr[0]
            wp_rr[0] = (wp_rr[0] + 1) % 4
            wv = arb(s).rearrange("p (a b) -> p a b", a=8)
            dma(wv, w_in_ab[:, col0:col0 + 128].rearrange("(kc p) f -> p kc f", p=128), [], kS(s), eng="pool")
            b = bank()
            for kc in range(8):
                mm(PS[:, b, 0:n], wv[:, kc, :], XNL[:, kc, 0:n], kc == 0, kc == 7, kS(s) + [kXL(kc)], [kP(b)])
            return b

        def proj_mix(rc, ti, o, n, dst, dkeys):
            b = proj(512 + rc * 128, ti, o, n)
            pt = arf(26)
            cp(pt[:, 0:n], PS[:, b, 0:n], [kP(b)], kS(26), eng="act")
            tm = arf(27)[:, 0:n]
            if ti < 4:
                ts1(tm[:, 1:n], pt[:, 0:n - 1], vcol("mu_shift", rc), ALU.mult, kS(26) + [kV], kS(27))
                ts1(tm[:, 0:1], PL[:, rc:rc + 1], vcol("mu_shift", rc), ALU.mult, [kSM("PL"), kV], kS(27))
            else:
                ts1(tm, SSH[:, rc, :], vcol("mu_shift", rc), ALU.mult, kS(18) + [kV], kS(27))
                cp(PSS[:, rc, :], pt[:, 0:n], kS(26), [kSM("PSS")])
            stt(dst, pt[:, 0:n], vcol("omu", rc), tm, ALU.mult, ALU.add, kS(26) + kS(27) + [kV], dkeys)
            if ti < 4:
                cp(PL[:, rc:rc + 1], pt[:, n - 1:n], kS(26), [kSM("PL")])

        fence(REGKEYS, WKEYS)
        for ti, (o, n) in enumerate(TT):
            RSTD_L[0] = 26
            rs = rms_stats(ti, o, n)
            for kc in range(8):
                stt(XNL[:, kc, 0:n], X[:, kc, o:o + n], vcol("norm_mix0", kc), rs, ALU.mult, ALU.mult,
                    [kX(kc, ti), kV] + kS(26), [kXL(kc)])
            RSTD_L[0] = 4
            if ti == 4:
                fence(WKEYS, REGKEYS)
                SP = arf(8, 4)
                for hb in range(2):
                    dma(SP[0:120, hb * 512:(hb + 1) * 512], spool[hb * 8:(hb + 1) * 8].rearrange("b r c -> (b r) c"), [], kS(8, 4))
                for hb in range(2):
                    for g in range(4):
                        b = bank()
                        mm(PS[:, b, 0:120], SP[0:120, hb * 512 + g * 128:hb * 512 + (g + 1) * 128], identF[0:120, 0:120], True, True,
                           kS(8, 4) + [kC], [kP(b)])
                        cp(SPT[:, g, hb * 8:(hb + 1) * 8, :], PS[:, b, 0:120].rearrange("p (b r) -> p b r", b=8), [kP(b)], kS(4, 2))
                dma(pool_s[:, 0:14, :], spool[:, 1:15, :], [], [("out", "pool_s")])
                dma(arf(8, 4)[0:16, 0:1792], sshift[:, :], kS(8, 4), kS(8, 4))
                b = bank()
                for rc in range(14):
                    mm(PS[:, b, rc * 16:(rc + 1) * 16], arf(8, 4)[0:16, rc * 128:(rc + 1) * 128], identF[0:16, 0:16], True, True,
                       kS(8, 4) + [kC], [kP(b)])
                cp(SSH, PS[:, b, 0:224].rearrange("p (c b) -> p c b", c=14), [kP(b)], kS(18))
            fence([kW("BR")], kS(14, 2))
            for g in range(4):
                w = POOL_W[g]
                b = proj(g * 128, ti, o, n)
                if ti < 4:
                    cp(PWK[:, 0:16], PC[:, g, :], [kSM("PC")], kS(14, 2))
                    cp(PWK[:, 16:16 + n], PS[:, b, 0:n], [kP(b)], kS(14, 2), eng="act")
                    cur, curk, lo, step = PWK, kS(14, 2), 0, 1
                    bufs = [(S_T1, arf(S_T1, 2)), (S_LW, arf(S_LW, 2))]
                    bi = 0
                    while step < w:
                        lo2 = lo + step
                        ds, dv = bufs[bi]
                        bi ^= 1
                        tt_(dv[:, lo2:16 + n], cur[:, lo2:16 + n], cur[:, lo2 - step:16 + n - step], ALU.add, curk, kS(ds, 2))
                        cur, curk, lo = dv, kS(ds, 2), lo2
                        step *= 2
                    stt(DPOOL[:, 0:n], cur[:, 16:16 + n], 1.0 / w, PWK[:, 16:16 + n], ALU.mult, ALU.subtract, curk + kS(14, 2), kS(27))
                    if ti == 0:
                        t16 = SM[:, SM_T0:SM_T0 + 16]
                        tt_(t16, cur[:, 16:32], CF[:, C_RC + g * 16:C_RC + (g + 1) * 16], ALU.mult, curk + [kC], [kSM("t16")])
                        tt_(DPOOL[:, 0:16], t16, PWK[:, 16:32], ALU.subtract, [kSM("t16")] + kS(14, 2), kS(27))
                    cp(PC[:, g, :], PWK[:, 512:528], kS(14, 2), [kSM("PC")])
                else:
                    cp(US[:, g, :], PS[:, b, 0:n], [kP(b)], [kSM("US")], eng="act")
                    t16 = SM[:, SM_T0:SM_T0 + 16]
                    red(t16, SPT[:, g, :, 16 - w:15], ALU.add, kS(4, 2), [kSM("t16")])
                    tt_(t16, t16, US[:, g, :], ALU.add, [kSM("t16"), kSM("US")], [kSM("t16")])
                    stt(DPOOL[:, 0:n], t16, 1.0 / w, US[:, g, :], ALU.mult, ALU.subtract, [kSM("t16"), kSM("US")], kS(27))
                b2 = bank()
                mm(PS[:, b2, 0:n], PW[:, g, :], DPOOL[:, 0:n], True, True, kS(12) + kS(27), [kP(b2)])
                act(A1[:, g, o:o + n], PS[:, b2, 0:n], AF.Identity, [kP(b2), kV], [kA1(g, ti)], scale=vcol("pool_scale", g))
            if ti == 3:
                stg = arf(S_T1)
                b = bank()
                for g in range(4):
                    mm(PS[0:15, b, g * 128:(g + 1) * 128], PC[:, g, 1:16], identF, True, True, [kSM("PC"), kC], [kP(b)])
                cp(stg[0:15, :], PS[0:15, b, :], [kP(b)], kS(S_T1))
                dma(pool_p[:, :], stg[0:15, :], kS(S_T1), [("out", "pool_p")])
            fence(kS(14, 2), [kW("BR")])
            m12 = arf(S_T1)[:, 0:n]
            proj_mix(12, ti, o, n, m12, kS(S_T1))
            act(TDA[0:64, 0:n], m12[0:64, :], AF.Tanh, kS(S_T1), kS(16))
            cp(TDA[64:128, 0:n], m12[64:128, :], kS(S_T1), kS(16))
            m13 = arf(S_T2)[:, 0:n]
            proj_mix(13, ti, o, n, m13, kS(S_T2))
            act(SGB[:, 0:n], m13, AF.Sigmoid, kS(S_T2), kS(16))
            for g in range(4):
                r_ = arf(S_R)[:, 0:n]; k_ = arf(S_K)[:, 0:n]; v_ = arf(S_V)[:, 0:n]
                a_ = arf(S_A)[:, 0:n]; g_ = arf(S_G)[:, 0:n]; lw = arf(S_LW)[:, 0:n]; kk = arf(S_KK)[:, 0:n]
                t1 = arf(S_T1)[:, 0:n]; t2 = arf(S_T2)[:, 0:n]; bon = arf(S_BON)[:, 0:n]; y_ = arf(S_Y)[:, 0:n]
                K = dict(R=kS(S_R), K=kS(S_K), V=kS(S_V), A=kS(S_A), LW=kS(S_LW), KK=kS(S_KK), T1=kS(S_T1), T2=kS(S_T2), Y=kS(S_Y))
                proj_mix(g, ti, o, n, r_, kS(S_R))
                proj_mix(4 + g, ti, o, n, k_, kS(S_K))
                proj_mix(8 + g, ti, o, n, v_, kS(S_V))
                b = bank()
                mm(PS[:, b, 0:n], W2A2[0:64, g * 128:(g + 1) * 128], TDA[0:64, 0:n], True, True, kS(13) + kS(16), [kP(b)])
                act(lw, PS[:, b, 0:n], AF.Sigmoid, [kP(b), kV], kS(S_LW), bias=vcol("rw_w0", g))
                ts1(lw, lw, -0.6065306597126334, ALU.mult, kS(S_LW), kS(S_LW))
                b = bank()
                mm(PS[:, b, 0:n], W2A2[64:128, g * 128:(g + 1) * 128], TDA[64:128, 0:n], True, True, kS(13) + kS(16), [kP(b)])
                act(a_, PS[:, b, 0:n], AF.Sigmoid, [kP(b), kV], kS(S_A), bias=vcol("rw_a0", g))
                b = bank()
                mm(PS[:, b, 0:n], G2[:, g * 128:(g + 1) * 128], SGB[:, 0:n], True, True, kS(13) + kS(16), [kP(b)])
                cp(g_, PS[:, b, 0:n], [kP(b)], kS(S_G), eng="act")
                ts1(kk, k_, vcol("rw_kk", g), ALU.mult, kS(S_K) + [kV], kS(S_KK))
                tt_(t1, kk, kk, ALU.mult, kS(S_KK), kS(S_T1))
                b = bank()
                mm(PS[:, b, 0:n], blkF, t1, True, True, kS(S_T1) + [kC], [kP(b)])
                ts1(t1, PS[:, b, 0:n], 1e-24, ALU.max, [kP(b)], kS(S_T1))
                act(t1, t1, AF.Ln, kS(S_T1), kS(S_T1))
                act(t1, t1, AF.Exp, kS(S_T1), kS(S_T1), scale=-0.5)
                tt_(kk, kk, t1, ALU.mult, kS(S_KK) + kS(S_T1), kS(S_KK))
                ts1(t1, a_, vcol("rw_ka", g), ALU.mult, kS(S_A) + [kV], kS(S_T1))
                ts1(t1, t1, vcol("omka", g), ALU.add, kS(S_T1) + [kV], kS(S_T1))
                tt_(k_, k_, t1, ALU.mult, kS(S_K) + kS(S_T1), kS(S_K))
                tt_(t1, r_, k_, ALU.mult, kS(S_R) + kS(S_K), kS(S_T1))
                ts1(t1, t1, vcol("rw_rk", g), ALU.mult, kS(S_T1) + [kV], kS(S_T1))
                b = bank()
                mm(PS[:, b, 0:n], blkF, t1, True, True, kS(S_T1) + [kC], [kP(b)])
                tt_(bon, PS[:, b, 0:n], v_, ALU.mult, [kP(b)] + kS(S_V), kS(S_BON))
                stt(a_, kk, -1.0, a_, ALU.mult, ALU.mult, kS(S_KK) + kS(S_A), kS(S_A))
                if ti < 4:
                    wkv_chunked(g, 0, r_, k_, v_, a_, lw, kk, t1, t2, y_, HS, K)
                else:
                    wkv_sample(g, r_, k_, v_, a_, lw, kk, y_, K)
                b = bank()
                mm(PS[:, b, 0:n], blkF, y_, True, True, kS(S_Y) + [kC], [kP(b)])
                stt(y_, PS[:, b, 0:n], -1.0 / 64.0, y_, ALU.mult, ALU.add, [kP(b)] + kS(S_Y), kS(S_Y))
                tt_(t1, y_, y_, ALU.mult, kS(S_Y), kS(S_T1))
                b = bank()
                mm(PS[:, b, 0:n], blkF, t1, True, True, kS(S_T1) + [kC], [kP(b)])
                act(t1, PS[:, b, 0:n], AF.Ln, [kP(b), kC], kS(S_T1), scale=1.0 / 64.0, bias=EPSL)
                act(t1, t1, AF.Exp, kS(S_T1), kS(S_T1), scale=-0.5)
                tt_(y_, y_, t1, ALU.mult, kS(S_Y) + kS(S_T1), kS(S_Y))
                ts1(y_, y_, vcol("rw_lnx_w", g), ALU.mult, kS(S_Y) + [kV], kS(S_Y))
                ts1(y_, y_, vcol("rw_lnx_b", g), ALU.add, kS(S_Y) + [kV], kS(S_Y))
                tt_(y_, y_, bon, ALU.add, kS(S_Y) + kS(S_BON), kS(S_Y))
                tt_(A1[:, 4 + g, o:o + n], y_, g_, ALU.mult, kS(S_Y) + kS(S_G), [kA1(4 + g, ti)])
        stg2 = arf(S_T2)
        b = bank()
        for g in range(4):
            mm(PS[0:16, b, g * 128:(g + 1) * 128], US[:, g, :], identF, True, True, [kSM("US"), kC], [kP(b)])
        cp(stg2[0:16, :], PS[0:16, b, :], [kP(b)], kS(S_T2))
        dma(pool_s[:, 14, :], stg2[0:16, :], kS(S_T2), [("out", "pool_s2")])
        b = bank()
        mm(PS[0:14, b, 0:128], PL, identF, True, True, [kSM("PL"), kC], [kP(b)])
        sp_ = arf(S_R)
        cp(sp_[0:14, 0:128], PS[0:14, b, 0:128], [kP(b)], kS(S_R))
        dma(shift_p[:, :], sp_[0:14, 0:128], kS(S_R), [("out", "shift_p")])
        ss_ = arf(28, 4)
        for grp in range(4):
            b = bank()
            rcs = list(range(grp * 4, min(14, grp * 4 + 4)))
            for j, rc in enumerate(rcs):
                mm(PS[0:16, b, j * 128:(j + 1) * 128], PSS[:, rc, :], identF, True, True, [kSM("PSS"), kC], [kP(b)])
            cp(ss_[0:16, grp * 512:grp * 512 + len(rcs) * 128], PS[0:16, b, 0:len(rcs) * 128], [kP(b)], kS(28, 4))
        dma(shift_s[:, :], ss_[0:16, 0:1792], kS(28, 4), [("out", "shift_s")])
        wst = arf(1, 2)[:, 0:512].rearrange("p (h k) -> p h k", h=8)
        b = bank()
        for g in range(4):
            mm(PS[0:64, b, g * 128:(g + 1) * 128], HS[:, g, :], identF, True, True, [kSM("HS"), kC], [kP(b)])
        cp(wst[0:64, :, :], PS[0:64, b, :].rearrange("p (h k) -> p h k", h=8), [kP(b)], kS(1, 2))
        dma(wkv_p.rearrange("h v k -> v h k"), wst[0:64, :, :], kS(1, 2), [("out", "wkv_p")])
        fence(WKEYS + [kSM("PSS"), kSM("US"), kSM("PC")] + [kXL(kc) for kc in range(8)], REGKEYS + [kSM("top")] + KXNALL0)
        out_proj(w_out_ab)

    def wkv_chunked(g, c0, r_, k_, v_, nkka, lw, kk, t1f, t2f, y_, HS, K):
        NCH = NC4
        sl = slice(c0, c0 + 64 * NCH)
        r_, k_, v_, nkka, lw, kk, t1, t2 = r_[:, sl], k_[:, sl], v_[:, sl], nkka[:, sl], lw[:, sl], kk[:, sl], t1f[:, sl], t2f[:, sl]
        c3 = lambda ap: ap.rearrange("p (c t) -> p c t", t=64)
        P.op("dve", lambda E: E.tensor_tensor_scan(t2, RSTM[:, 0:64 * NCH], lw, 0.0, ALU.mult, ALU.add), K["LW"] + [kCB], K["T2"])
        tt_(t1, t2, lw, ALU.subtract, K["T2"] + K["LW"], K["T1"])
        act(t1, t1, AF.Exp, K["T1"], K["T1"])
        tt_(t1, t1, kk, ALU.mult, K["T1"] + K["KK"], K["T1"])
        bm4 = bmB.rearrange("p (h t) -> p h t", h=2).unsqueeze(1).broadcast_to([128, NCH, 2, 64])

        def to_bp(dst3, src, skeys, dkey):
            tt_(dst3.rearrange("p c (h t) -> p c h t", h=2), c3(src).unsqueeze(2).broadcast_to([128, NCH, 2, 64]), bm4, ALU.mult,
                skeys + [kCB], [dkey])
        to_bp(WBR[:, :, 0:128], t1, K["T1"], kW("BR"))
        act(t1, t2, AF.Exp, K["T2"], K["T1"])
        tt_(WBR[:, :, 128:192], c3(r_), c3(t1), ALU.mult, K["R"] + K["T1"], [kW("BR")])
        plc = SM[:, 912:912 + NCH]
        cp(plc, c3(t1)[:, :, 63], K["T1"], [kSM("plc")])
        act(t2, t2, AF.Exp, K["T2"], K["T2"], scale=-1.0)
        tt_(t1, nkka, t2, ALU.mult, K["A"] + K["T2"], K["T1"])
        to_bp(WA, t1, K["T1"], kW("A"))
        tt_(t1, k_, t2, ALU.mult, K["K"] + K["T2"], K["T1"])
        to_bp(WK, t1, K["T1"], kW("K"))
        to_bp(WV, v_, K["V"], kW("V"))
        ev = [0]

        def ee_():
            ev[0] ^= 1
            return "act" if ev[0] else "dve"
        for c in range(NCH):
            b = bank()
            mm(PS[:, b, 0:128], WA[:, c, :], identB, True, True, [kW("A"), kCB], [kP(b)])
            cp(WAp[:, c, :], PS[:, b, 0:128], [kP(b)], [kW("Ap")], eng=ee_())
            b = bank()
            mm(PS[:, b, 0:128], WK[:, c, :], identB, True, True, [kW("K"), kCB], [kP(b)])
            cp(WKp[:, c, :], PS[:, b, 0:128], [kP(b)], [kW("Kp")], eng=ee_())
            b = bank()
            mm(PS[:, b, 0:128], WBR[:, c, 0:128], identB, True, True, [kW("BR"), kCB], [kP(b)])
            cp(WX1[:, c, 0:128], PS[:, b, 0:128], [kP(b)], [kW("X1")], eng=ee_())
            b = bank()
            mm(PS[:, b, 0:192], WV[:, c, :], IEB, True, True, [kW("V"), kCB], [kP(b)])
            cp(WVV[:, c, :], PS[:, b, 0:192], [kP(b)], [kW("VV")], eng=ee_())
        for c in range(NCH):
            b = bank()
            mm(PS[:, b, 0:192], WA[:, c, :], WBR[:, c, :], True, True, [kW("A"), kW("BR")], [kP(b)])
            tt_(WZA[:, c, :], PS[:, b, 0:192], mskB, ALU.mult, [kP(b), kCB], [kW("ZA")])
            b = bank()
            mm(PS[:, b, 0:192], WK[:, c, :], WBR[:, c, :], True, True, [kW("K"), kW("BR")], [kP(b)])
            tt_(WZK[:, c, :], PS[:, b, 0:192], mskB, ALU.mult, [kP(b), kCB], [kW("ZK")])
            b = bank()
            mm(PS[:, b, 0:128], WBR[:, c, 0:128], WA[:, c, :], True, True, [kW("A"), kW("BR")], [kP(b)])
            tt_(WNT[:, c, 0:128], PS[:, b, 0:128], mslB, ALU.mult, [kP(b), kCB], [kW("NQ")])
        for c in range(NCH):
            cp(WNQ[:, c, 0:128], WZA[:, c, 0:128], [kW("ZA")], [kW("NQ")], eng="act")
            tt_(WNQ[:, c, 128:256], WZA[:, c, 0:128], identB, ALU.add, [kW("ZA"), kCB], [kW("NQ")])
            P.op("pool", lambda E, c=c: E.tensor_tensor(WNT[:, c, 128:256], WNT[:, c, 0:128], identB, ALU.add), [kW("NQ"), kCB], [kW("NQ")])
        for t in range(1, 7):
            for c in range(NCH):
                NTj = W4[:, c, 256:384]; NTIj = W4[:, c, 384:512]; Nj = W4[:, c, 0:128]; Qj = W4[:, c, 128:256]
                b = bank()
                kq = [kW("NQ")]
                eng = "act" if c % 2 == 0 else "dve"
                if t <= 5:
                    mm(PS[:, b, 0:128], NTj, Nj, True, True, kq, [kP(b)])
                    if t > 1:
                        mm(PS[:, b, 128:256], NTIj, Qj, True, True, kq, [kP(b)])
                    else:
                        mm(PS[:, b, 128:256], identB, Qj, True, True, kq + [kCB], [kP(b)])
                    mm(PS[:, b, 256:384], Nj, NTj, True, True, kq, [kP(b)])
                    mm(PS[:, b, 384:512], Nj, NTj, True, False, kq, [kP(b)])
                    mm(PS[:, b, 384:512], identB, identB, False, True, [kCB], [kP(b)])
                    if eng == "act":
                        cp(W4[:, c, :], PS[:, b, :], [kP(b)], [kW("NQ")], eng="act")
                    else:
                        ts1(W4[:, c, :], PS[:, b, :], 1.0, ALU.mult, [kP(b)], [kW("NQ")])
                else:
                    mm(PS[:, b, 0:128], NTIj, Qj, True, True, kq, [kP(b)])
                    if eng == "act":
                        cp(W4[:, c, 128:256], PS[:, b, 0:128], [kP(b)], [kW("NQ")], eng="act")
                    else:
                        ts1(W4[:, c, 128:256], PS[:, b, 0:128], 1.0, ALU.mult, [kP(b)], [kW("NQ")])
        for c in range(NCH):
            b = bank()
            mm(PS[:, b, 0:192], WZK[:, c, 0:128], WVV[:, c, :], True, True, [kW("ZK"), kW("VV")], [kP(b)])
            cp(WX1[:, c, 128:320], PS[:, b, 0:192], [kP(b)], [kW("X1")], eng=ee_())
        for c in range(NCH):
            b = bank()
            mm(PS[:, b, 0:320], WNQ[:, c, 128:256], WX1[:, c, :], True, True, [kW("NQ"), kW("X1")], [kP(b)])
            cp(WBU[:, c, :], PS[:, b, 0:320], [kP(b)], [kW("BU")], eng=ee_())
        for c in range(NCH):
            b = bank()
            mm(PS[:, b, 0:128], WBU[:, c, 0:128], WAp[:, c, :], True, True, [kW("BU"), kW("Ap")], [kP(b)])
            tt_(WGT[:, c, :], PS[:, b, 0:128], identF, ALU.add, [kP(b), kC], [kW("GT")])
            b = bank()
            mm(PS[:, b, 0:64], WAp[:, c, :], WBU[:, c, 256:320], True, False, [kW("BU"), kW("Ap")], [kP(b)])
            mm(PS[:, b, 0:64], WKp[:, c, :], WVV[:, c, 128:192], False, True, [kW("Kp"), kW("VV")], [kP(b)])
            ts1(WCC[:, c, :], PS[:, b, 0:64], plc[:, c:c + 1], ALU.mult, [kP(b), kSM("plc")], [kW("CC")])
            b = bank()
            mm(PS[:, b, 0:64], WBU[:, c, 0:128], WZA[:, c, 128:192], True, True, [kW("BU"), kW("ZA")], [kP(b)])
            tt_(WRH[:, c, :], PS[:, b, 0:64], WBR[:, c, 128:192], ALU.add, [kP(b), kW("BR")], [kW("RH")])
        for c in range(NCH):
            hp = WHP[:, c % 2, :]
            tt_(hp.rearrange("p (h v) -> p h v", h=2), HS[:, g, :].unsqueeze(1).broadcast_to([128, 2, 64]),
                bmB.rearrange("p (h t) -> p h t", h=2), ALU.mult, [kSM("HS"), kCB], [kW("HP%d" % (c % 2))])
            b = bank()
            mm(PS[:, b, 0:64], WBU[:, c, 128:256], WZA[:, c, 128:192], True, False, [kW("BU"), kW("ZA")], [kP(b)])
            mm(PS[:, b, 0:64], WVV[:, c, 0:128], WZK[:, c, 128:192], False, False, [kW("VV"), kW("ZK")], [kP(b)])
            mm(PS[:, b, 0:64], hp, WRH[:, c, :], False, True, [kW("HP%d" % (c % 2)), kW("RH")], [kP(b)])
            cp(y_[:, c0 + c * 64:c0 + (c + 1) * 64], PS[:, b, 0:64], [kP(b)], K["Y"], eng="act")
            b = bank()
            mm(PS[:, b, 0:64], WGT[:, c, :], HS[:, g, :], True, True, [kW("GT"), kSM("HS")], [kP(b)])
            stt(HS[:, g, :], PS[:, b, 0:64], plc[:, c:c + 1], WCC[:, c, :], ALU.mult, ALU.add,
                [kP(b), kSM("plc"), kW("CC")], [kSM("HS")])

    def wkv_sample(g, r_, k_, v_, nkka, lw, kk, y_, K):
        SW = arf(4, 2).rearrange("p (b k) -> p b k", b=16)
        T1 = arf(6, 2).rearrange("p (b k) -> p b k", b=16)
        T2 = arf(8, 2).rearrange("p (b k) -> p b k", b=16)
        RQ = arf(10, 2).rearrange("p (b k) -> p b k", b=16)
        kSW, kT1, kT2, kRQ = kS(4, 2), kS(6, 2), kS(8, 2), kS(10, 2)
        for hh_ in range(2):
            dma(SW[hh_ * 64:(hh_ + 1) * 64], swkv[:, 2 * g + hh_, :, :].rearrange("b v k -> v b k"), [], kSW)
        act(lw, lw, AF.Exp, K["LW"], K["LW"])
        I2 = EF.unsqueeze(1).broadcast_to([128, 16, 64])

        def bcast(q, qk):
            tt_(RQ, q.unsqueeze(2).broadcast_to([128, 16, 64]), I2, ALU.mult, qk + [kC], kRQ)
            b0, b1 = bank2()
            for hf, bb in ((0, b0), (1, b1)):
                mm(PS[:, bb, :], blkF, arf(10, 2)[:, hf * 512:(hf + 1) * 512], True, True, kRQ + [kC], [kP(bb)])
            return PS[:, b0:b0 + 2, :].rearrange("p a (b k) -> p (a b) k", k=64), [kP(b0), kP(b1)]
        skk = SM[:, SM_T0:SM_T0 + 16]
        bc, bk = bcast(kk, K["KK"])
        tt_(T1, SW, bc, ALU.mult, kSW + bk, kT1)
        red(skk, T1, ALU.add, kT1, [kSM("skk")])
        bc, bk = bcast(lw, K["LW"])
        tt_(T2, SW, bc, ALU.mult, kSW + bk, kT2)
        bc, bk = bcast(nkka, K["A"])
        tt_(T1, bc, skk.unsqueeze(2).broadcast_to([128, 16, 64]), ALU.mult, bk + [kSM("skk")], kT1)
        tt_(T2, T2, T1, ALU.add, kT2 + kT1, kT2)
        bc, bk = bcast(k_, K["K"])
        tt_(T1, bc, v_.unsqueeze(2).broadcast_to([128, 16, 64]), ALU.mult, bk + K["V"], kT1)
        tt_(T2, T2, T1, ALU.add, kT2 + kT1, kT2)
        bc, bk = bcast(r_, K["R"])
        tt_(T1, T2, bc, ALU.mult, kT2 + bk, kT1)
        red(y_, T1, ALU.add, kT1, K["Y"])
        for hh_ in range(2):
            dma(wkv_s[:, 2 * g + hh_, :, :].rearrange("b v k -> v b k"), T2[hh_ * 64:(hh_ + 1) * 64], kT2, [("out", "wkv_s", g, hh_)])

    def l1_mixer():
        rmsnorm("norm_mix1")
        SCT = SM[:, SM_SCT:SM_SCT + 256].rearrange("p (j b r) -> p j b r", j=8, b=16)
        CTc = SM[:, SM_CT:SM_CT + 16].rearrange("p (j r) -> p j r", j=8)
        CSs = SM[:, SM_CS:SM_CS + 128].rearrange("p (j b) -> p j b", j=8)
        stg = arf(5, 2)
        dma(stg[0:32, 0:1024], sconv.rearrange("b r c -> (b r) c"), [], kS(5, 2))
        b = bank()
        for j in range(8):
            mm(PS[:, b, j * 32:(j + 1) * 32], stg[0:32, j * 128:(j + 1) * 128], identF[0:32, 0:32], True, True, kS(5, 2) + [kC], [kP(b)])
        cp(SM[:, SM_SCT:SM_SCT + 256], PS[:, b, 0:256], [kP(b)], [kSM("SCT")])
        dma(conv_s[:, 0, :], sconv[:, 1, :], [], [("out", "conv_s0")])
        CT = arf(7, 2)
        BG, CG, ZZ = 9, 10, 11
        for j in range(8):
            s0 = 12 + (j % 2) * 3
            wv = arb(s0, 3).rearrange("p (w kc f) -> p w kc f", w=3, kc=8)
            ks = kS(s0, 3)
            for wi in range(3):
                dma(wv[:, wi], w_in_c[:, wi * 1024 + j * 128:wi * 1024 + (j + 1) * 128].rearrange("(kc p) f -> p kc f", p=128), [], ks, eng="pool")
            memset(CT[:, 0:2], 0.0, kS(7, 2))
            for ti, (o, n) in enumerate(TT):
                bs = []
                for wi in range(3):
                    b = bank()
                    for kc in range(8):
                        mm(PS[:, b, 0:n], wv[:, wi, kc, :], XN[:, kc, o:o + n], kc == 0, kc == 7, ks + [kXN(kc, ti)], [kP(b)])
                    bs.append(b)
                bg = arf(BG)[:, 0:n]; cg = arf(CG)[:, 0:n]; zz = arf(ZZ)[:, 0:n]
                cp(bg, PS[:, bs[0], 0:n], [kP(bs[0])], kS(BG), eng="act")
                cp(cg, PS[:, bs[1], 0:n], [kP(bs[1])], kS(CG), eng="act")
                if ti < 4:
                    tt_(CT[:, 2:2 + n], PS[:, bs[2], 0:n], cg, ALU.mult, [kP(bs[2])] + kS(CG), kS(7, 2))
                    ts1(zz, CT[:, 0:n], vcol("conv_w0", j), ALU.mult, kS(7, 2) + [kV], kS(ZZ))
                    stt(zz, CT[:, 1:1 + n], vcol("conv_w1", j), zz, ALU.mult, ALU.add, kS(7, 2) + kS(ZZ) + [kV], kS(ZZ))
                    stt(zz, CT[:, 2:2 + n], vcol("conv_w2", j), zz, ALU.mult, ALU.add, kS(7, 2) + kS(ZZ) + [kV], kS(ZZ))
                    tt_(A1[:, j, o:o + n], bg, zz, ALU.mult, kS(BG) + kS(ZZ), [kA1(j, ti)])
                    cp(SM[:, SM_T0 + 14:SM_T0 + 16], CT[:, n:n + 2], kS(7, 2), [kSM("ctt")])
                    cp(CT[:, 0:2], SM[:, SM_T0 + 14:SM_T0 + 16], [kSM("ctt")], kS(7, 2))
                    if ti == 3:
                        cp(CTc[:, j, :], CT[:, 0:2], kS(7, 2), [kSM("CT")])
                else:
                    tt_(CSs[:, j, :], PS[:, bs[2], 0:n], cg, ALU.mult, [kP(bs[2])] + kS(CG), [kSM("CS")])
                    ts1(zz, SCT[:, j, :, 0], vcol("conv_w0", j), ALU.mult, [kSM("SCT"), kV], kS(ZZ))
                    stt(zz, SCT[:, j, :, 1], vcol("conv_w1", j), zz, ALU.mult, ALU.add, [kSM("SCT"), kV] + kS(ZZ), kS(ZZ))
                    stt(zz, CSs[:, j, :], vcol("conv_w2", j), zz, ALU.mult, ALU.add, [kSM("CS"), kV] + kS(ZZ), kS(ZZ))
                    tt_(A1[:, j, o:o + n], bg, zz, ALU.mult, kS(BG) + kS(ZZ), [kA1(j, ti)])
        for hf in range(2):
            b = bank()
            for jj in range(4):
                mm(PS[0:2, b, jj * 128:(jj + 1) * 128], CTc[:, hf * 4 + jj, :], identF, True, True, [kSM("CT"), kC], [kP(b)])
            cp(arf(18 + hf)[0:2, :], PS[0:2, b, :], [kP(b)], kS(18 + hf))
            dma(conv_p[:, hf * 512:(hf + 1) * 512], arf(18 + hf)[0:2, :], kS(18 + hf), [("out", "conv_p", hf)])
            b = bank()
            for jj in range(4):
                mm(PS[0:16, b, jj * 128:(jj + 1) * 128], CSs[:, hf * 4 + jj, :], identF, True, True, [kSM("CS"), kC], [kP(b)])
            cp(arf(29 + hf)[0:16, :], PS[0:16, b, :], [kP(b)], kS(29 + hf))
            dma(conv_s[:, 1, hf * 512:(hf + 1) * 512], arf(29 + hf)[0:16, :], kS(29 + hf), [("out", "conv_s1", hf)])
        out_proj(w_out_c)

    def final():
        YF = arf(8, 8).rearrange("p (a b) -> p a b", a=8)
        for ti, (o, n) in enumerate(TT):
            rs = rms_stats(ti, o, n)
            for kc in range(8):
                stt(YF[:, kc, 0:n], X[:, kc, o:o + n], vcol("norm_final", kc), rs, ALU.mult, ALU.mult,
                    [kX(kc, ti), kV] + kS(RSTD), kS(8, 8))
            nb = (n + 127) // 128
            for bi in range(nb):
                m = min(128, n - bi * 128)
                so = 16 + (bi % 2) * 2
                og = arf(so, 2)
                for hf in range(2):
                    b = bank()
                    for j in range(4):
                        kc = hf * 4 + j
                        mm(PS[0:m, b, j * 128:(j + 1) * 128], YF[:, kc, bi * 128:bi * 128 + m], identF, True, True,
                           kS(8, 8) + [kC], [kP(b)])
                    cp(og[0:m, hf * 512:(hf + 1) * 512], PS[0:m, b, :], [kP(b)], kS(so, 2), eng=("act" if hf else "dve"))
                if ti < 4:
                    dma(y_prompt[o + bi * 128:o + bi * 128 + m, :], og[0:m, :], kS(so, 2), [("out", "y", ti, bi)])
                else:
                    dma(y_sample[0:16, :], og[0:16, :], kS(so, 2), [("out", "ys")])

    l0_mixer_run()
    if dbg_stage >= 2:
        xattn(0)
    if dbg_stage >= 3:
        ffn_dense()
    if dbg_stage >= 4:
        l1_mixer()
    if dbg_stage >= 5:
        xattn(1)
    if dbg_stage >= 6:
        moe()
    final()
    P.emit()
    st.close()
    nc._used_inputs = list(USED)
    nc._prog_stats = (P.sig_counts, P.n_ops, list(P.dma_cnt))
    return nc


_CACHE = {}


def _get_nc(dbg_stage=99):
    if dbg_stage not in _CACHE:
        nc = bass.Bass("TRN2", target_bir_lowering=False)
        build_program(nc, dbg_stage)
        _CACHE[dbg_stage] = nc
    return _CACHE[dbg_stage]


def kernel(**inp):
    dbg_stage = int(inp.pop("_dbg_stage", 99))
    f = lambda a: np.ascontiguousarray(np.asarray(a, dtype=np.float32))
    nc = _get_nc(dbg_stage)
    vecs = _pack_vecs(inp)
    consts, constsb = _consts()
    shared = dict(
        w_xq=f(inp["w_xq"]), w_xk=f(inp["w_xk"]), w_xv=f(inp["w_xv"]), w_xo=f(inp["w_xo"]),
        w_in_ab=f(inp["w_in_ab"][0]), pool_w=f(inp["pool_w"][0]),
        rw_w2=f(inp["rw_w2"][0]), rw_a2=f(inp["rw_a2"][0]), rw_g2=f(inp["rw_g2"][0]),
        w_out_ab=f(inp["w_out_ab"][0]),
        ffn_gate=f(inp["ffn_gate"][0]), ffn_up=f(inp["ffn_up"][0]), ffn_down=f(inp["ffn_down"][0]),
        w_in_c=f(inp["w_in_c"][0]), w_out_c=f(inp["w_out_c"][0]), router_w=f(inp["router_w"][0]),
        moe_gate=f(inp["moe_gate"][0]), moe_up=f(inp["moe_up"][0]), moe_down=f(inp["moe_down"][0]),
        vecs=vecs, consts=consts, constsb=constsb,
    )
    in_maps = []
    for c in range(8):
        s = slice(16 * c, 16 * (c + 1))
        m = dict(shared)
        m.update(
            x_prompt=f(inp["x_prompt"][c]), x_sample=f(inp["x_sample"][s, 0]), mem_prompt=f(inp["mem_prompt"][c]),
            cache_mem_k=f(inp["cache_mem_k"][:, s].reshape(2, 16, 256, 1024)),
            cache_mem_v=f(inp["cache_mem_v"][:, s].reshape(2, 16, 256, 1024)),
            state_pool=f(inp["state_pool"][0, s]), state_shift=f(inp["state_shift"][0, s]),
            state_wkv=f(inp["state_wkv"][0, s]), state_conv=f(inp["state_conv"][0, s]),
        )
        in_maps.append(m)
    used = set(nc._used_inputs)
    in_maps = [{k: v for k, v in m.items() if k in used} for m in in_maps]
    res = run_bass_kernel_spmd(nc, in_maps, core_ids=list(range(8)))
    R = res.results
    cat = lambda k: np.stack([np.asarray(R[c][k], np.float32) for c in range(8)])
    y_prompt = cat("y_prompt")
    y_sample = np.concatenate([R[c]["y_sample"] for c in range(8)], 0).reshape(128, 1, 1024)
    pool_p = cat("pool_p")[None]
    pool_s = np.concatenate([R[c]["pool_s"] for c in range(8)], 0)[None]
    shift_p = cat("shift_p").reshape(8, 1792)[None]
    shift_s = np.concatenate([R[c]["shift_s"] for c in range(8)], 0)[None]
    wkv_p = cat("wkv_p")[None]
    wkv_s = np.concatenate([R[c]["wkv_s"] for c in range(8)], 0)[None]
    conv_p = cat("conv_p")[None]
    conv_s = np.concatenate([R[c]["conv_s"] for c in range(8)], 0)[None]
    mem_k_p = np.stack([R[c]["mem_k_p"] for c in range(8)], 1).reshape(2, 8, 256, 4, 256)
    mem_v_p = np.stack([R[c]["mem_v_p"] for c in range(8)], 1).reshape(2, 8, 256, 4, 256)
    outs = (y_prompt, y_sample, pool_p, pool_s, shift_p, shift_s, wkv_p, wkv_s, conv_p, conv_s, mem_k_p, mem_v_p)
    return tuple(np.ascontiguousarray(o, dtype=np.float32) for o in outs)
```

```python
import numpy as np
import contextlib
import os
SKIP = os.environ.get('KSKIP', '')
import concourse.bass as bass
import concourse.mybir as mybir
from concourse.bass_utils import run_bass_kernel_spmd

F32 = mybir.dt.float32
BF16 = mybir.dt.bfloat16
AF = mybir.ActivationFunctionType
ALU = mybir.AluOpType
AX = mybir.AxisListType

NT = 2064
TT = [(0, 512), (512, 512), (1024, 512), (1536, 512), (2048, 16)]
D = 1024
NSLOT = 34
RMS_EPS = 1e-6
LNX_EPS = 64e-5
POOL_W = (2, 4, 8, 16)

VEC_COLS = {}


def _vec_layout():
    off = 0
    def add(name, n):
        nonlocal off
        VEC_COLS[name] = off
        off += n
    for nm in ("norm_mix", "norm_xattn", "norm_mem", "norm_ffn"):
        add(nm + "0", 8)
        add(nm + "1", 8)
    add("norm_final", 8)
    add("pool_scale", 4)
    add("mu_shift", 14)
    for nm in ("rw_w0", "rw_a0", "rw_kk", "rw_ka", "rw_rk", "rw_lnx_w", "rw_lnx_b"):
        add(nm, 4)
    add("conv_w0", 8)
    add("conv_w1", 8)
    add("conv_w2", 8)
    add("router_b", 1)
    add("omu", 14)
    add("omka", 4)
    return off


NVEC = _vec_layout()

C_ID = 0
C_BLK = 128
C_E = 256
C_RC = 320
C_ONE = 384
C_EPS = 512
C_IOTA = 520
NCONST = 648
B_ID = 0; B_ONE = 128; B_BM = 256; B_IE = 384; B_MSU = 576; B_MSL = 768; B_RST = 896
NCB = 1408


def _consts():
    c = np.zeros((128, NCONST), np.float32)
    cb = np.zeros((128, NCB), np.float32)
    p = np.arange(128)
    c[:, C_ID:C_ID + 128] = np.eye(128, dtype=np.float32)
    hh = p // 64
    c[:, C_BLK:C_BLK + 128] = (hh[:, None] == hh[None, :]).astype(np.float32)
    bm = np.zeros((128, 2, 64), np.float32)
    bm[p, hh, :] = 1.0
    s = p % 64
    t = np.arange(64)
    su = (t[None, :] > s[:, None]).astype(np.float32)
    ui = (t[None, :] >= s[:, None]).astype(np.float32)
    sl = (t[None, :] < s[:, None]).astype(np.float32)
    E = (s[:, None] == t[None, :]).astype(np.float32)
    c[:, C_E:C_E + 64] = E
    for g, w in enumerate(POOL_W):
        c[:, C_RC + g * 16:C_RC + (g + 1) * 16] = 1.0 / np.minimum(w, np.arange(16) + 1.0)
    c[:, C_ONE:C_ONE + 128] = 1.0
    c[:, C_EPS] = RMS_EPS
    c[:, C_EPS + 1] = LNX_EPS
    c[:, C_IOTA:C_IOTA + 128] = np.arange(128, dtype=np.float32)[None, :]
    cb[:, B_ID:B_ID + 128] = np.eye(128, dtype=np.float32)
    cb[:, B_ONE:B_ONE + 128] = 1.0
    cb[:, B_BM:B_BM + 128] = bm.reshape(128, 128)
    cb[:, B_IE:B_IE + 128] = np.eye(128, dtype=np.float32)
    cb[:, B_IE + 128:B_IE + 192] = E
    cb[:, B_MSU:B_MSU + 128] = (bm * su[:, None, :]).reshape(128, 128)
    cb[:, B_MSU + 128:B_MSU + 192] = ui
    cb[:, B_MSL:B_MSL + 128] = (bm * sl[:, None, :]).reshape(128, 128)
    rst = np.ones(512, np.float32)
    rst[::64] = 0.0
    cb[:, B_RST:B_RST + 512] = rst[None, :]
    return c, cb


def _pack_vecs(inp):
    v = np.zeros((128, NVEC), np.float32)
    def put(name, arr):
        a = np.asarray(arr, np.float32).reshape(-1)
        n = a.size // 128
        v[:, VEC_COLS[name]:VEC_COLS[name] + n] = a.reshape(n, 128).T
    for nm in ("norm_mix", "norm_xattn", "norm_mem", "norm_ffn"):
        put(nm + "0", inp[nm][0])
        put(nm + "1", inp[nm][1])
    put("norm_final", inp["norm_final"])
    put("pool_scale", inp["pool_scale"][0])
    put("mu_shift", inp["mu_shift"][0])
    for nm in ("rw_w0", "rw_a0", "rw_kk", "rw_ka", "rw_rk", "rw_lnx_w", "rw_lnx_b"):
        put(nm, inp[nm][0])
    for j in range(3):
        put("conv_w%d" % j, inp["conv_w"][0, j])
    v[0:8, VEC_COLS["router_b"]] = np.asarray(inp["router_b"], np.float32).reshape(8)
    return v


class Prog:
    def __init__(self, nc):
        self.nc = nc
        self.ops = []
        self.res = {}
        self.ndma = 28
        self.dma_last = [None] * self.ndma
        self.dma_cnt = [0] * self.ndma
        self.rr = 0
        self.cur_region = None
        self.regions = []
        self.flag_ap = None
        self.flag_key = None

    def region_begin(self, sense):
        self.regions.append(dict(sense=sense))
        self.cur_region = len(self.regions) - 1

    def region_end(self):
        self.cur_region = None

    def flagload(self, engs, keys):
        for e in engs:
            i = self.op(e, None, keys, ())
            self.ops[i]["flagload"] = True

    def op(self, eng, fn, r=(), w=(), dma=False):
        i = len(self.ops)
        deps = {}
        for k in r:
            e = self.res.get(k)
            if e is not None and e[0] is not None:
                deps[e[0]] = "raw"
        for k in w:
            e = self.res.get(k)
            if e is not None:
                if e[0] is not None:
                    deps.setdefault(e[0], "waw")
                for rd in e[1]:
                    deps.setdefault(rd, "war")
        o = dict(eng=eng, fn=fn, dma=dma, deps=deps, sig=False, val=0, region=self.cur_region)
        if dma:
            j = self.rr
            self.rr = (self.rr + 1) % self.ndma
            if self.dma_last[j] is not None:
                deps[self.dma_last[j]] = "raw"
            self.dma_cnt[j] += 16
            o["sem"] = j
            o["dval"] = self.dma_cnt[j]
            self.dma_last[j] = i
        deps.pop(i, None)
        self.ops.append(o)
        for k in r:
            self.res.setdefault(k, [None, []])[1].append(i)
        for k in w:
            self.res[k] = [i, []]
        return i

    def emit(self):
        nc = self.nc
        ops = self.ops
        engs = ["pe", "act", "dve", "pool", "sp"]
        for o in ops:
            waits = []
            for d, kind in o["deps"].items():
                do = ops[d]
                if do["dma"]:
                    waits.append(d)
                elif do["eng"] == o["eng"] and not o["dma"]:
                    if o["eng"] != "pe" and kind == "raw":
                        waits.append(d)
                else:
                    waits.append(d)
            o["waits"] = waits
        for e in engs:
            widx = {}
            saved = None
            cur = None
            for o in ops:
                if o["eng"] != e:
                    continue
                rg = o["region"]
                if rg != cur:
                    if cur is not None:
                        widx = saved
                    if rg is not None:
                        saved = dict(widx)
                    cur = rg
                kept = []
                for d in sorted(o["waits"], reverse=True):
                    do = ops[d]
                    key = ("d", do["sem"]) if do["dma"] else do["eng"]
                    if widx.get(key, -1) >= d:
                        continue
                    widx[key] = d
                    kept.append(d)
                o["waits"] = kept
                for d in kept:
                    if not ops[d]["dma"]:
                        ops[d]["sig"] = True
        cnt = {e: 0 for e in engs}
        for o in ops:
            if o["sig"]:
                cnt[o["eng"]] += 1
                o["val"] = cnt[o["eng"]]
        self.sig_counts = dict(cnt)
        self.n_ops = {e: sum(1 for o in ops if o['eng'] == e) for e in engs}
        with contextlib.ExitStack() as st:
            sems = {e: st.enter_context(nc.semaphore("s_" + e)) for e in engs}
            dsem = [st.enter_context(nc.semaphore("d%d" % j)) for j in range(self.ndma)]
            block = st.enter_context(nc.Block())
            dma_cnt = self.dma_cnt

            nreg = len(self.regions)
            comp = [{e: 0 for e in engs} for _ in range(nreg)]
            dcomp = [{e: {} for e in engs} for _ in range(nreg)]
            for o in ops:
                rg = o["region"]
                if rg is None:
                    continue
                if o["dma"]:
                    dd = dcomp[rg][o["eng"]]
                    dd[o["sem"]] = dd.get(o["sem"], 0) + 16
                elif o["sig"]:
                    comp[rg][o["eng"]] += 1
            flag_ap = self.flag_ap

            def run(ename, E):
                waited = {}
                saved = None
                cur = None
                guard = None
                reg = None
                fval = None
                involved = set(o["region"] for o in ops if o["eng"] == ename and o["region"] is not None)

                def close_region():
                    nonlocal guard, cur, waited, saved
                    guard.__exit__(None, None, None)
                    els = E.Else()
                    els.__enter__()
                    E.drain()
                    if comp[cur][ename]:
                        E.sem_inc(sems[ename], comp[cur][ename])
                    for j, v in dcomp[cur][ename].items():
                        E.sem_inc(dsem[j], v)
                    els.__exit__(None, None, None)
                    waited = saved
                    guard = None
                    cur = None

                for o in ops:
                    if o["eng"] != ename:
                        continue
                    rg = o["region"]
                    if rg != cur:
                        if cur is not None:
                            close_region()
                        if rg is not None:
                            assert reg is not None, "flag not loaded on " + ename
                            saved = dict(waited)
                            guard = E.If(fval > 0) if self.regions[rg]["sense"] else E.If(fval == 0)
                            guard.__enter__()
                            cur = rg
                    for d in o["waits"]:
                        do = ops[d]
                        if do["dma"]:
                            key, val, sm = ("d", do["sem"]), do["dval"], dsem[do["sem"]]
                        else:
                            key, val, sm = do["eng"], do["val"], sems[do["eng"]]
                        E.wait_ge(sm, val)
                    if o.get("flagload"):
                        reg = E.alloc_register("flag_" + ename)
                        E.reg_load(reg, flag_ap)
                        fval = E.snap(reg)
                        continue
                    ins = o["fn"](E)
                    if o["dma"]:
                        ins.then_inc(dsem[o["sem"]], 16)
                    elif o["sig"]:
                        ins.then_inc(sems[ename], 1)
                if cur is not None:
                    close_region()
                if ename == "sp":
                    for j in range(self.ndma):
                        if dma_cnt[j] > 0:
                            E.wait_ge(dsem[j], dma_cnt[j])

            @block.tensor
            def _(E):
                run("pe", E)

            @block.scalar
            def _(E):
                run("act", E)

            @block.vector
            def _(E):
                run("dve", E)

            @block.gpsimd
            def _(E):
                run("pool", E)

            @block.sync
            def _(E):
                run("sp", E)


def build_program(nc, dbg_stage=99):
    P = Prog(nc)
    st = contextlib.ExitStack()

    USED = []
    NEED = {"moe_gate": 6, "moe_up": 6, "moe_down": 6, "router_w": 6, "ffn_gate": 3, "ffn_up": 3, "ffn_down": 3,
            "w_in_c": 4, "w_out_c": 4, "state_conv": 4, "w_xq": 2, "w_xk": 2, "w_xv": 2, "w_xo": 2,
            "cache_mem_k": 2, "cache_mem_v": 2, "mem_prompt": 2}

    def din(name, shape):
        if dbg_stage < NEED.get(name, 0):
            return None
        USED.append(name)
        return nc.dram_tensor(name, list(shape), F32, kind="ExternalInput").ap()

    def dout(name, shape):
        return nc.dram_tensor(name, list(shape), F32, kind="ExternalOutput").ap()

    xp = din("x_prompt", [2048, D])
    xs = din("x_sample", [16, D])
    memp = din("mem_prompt", [256, D])
    ck = din("cache_mem_k", [2, 16, 256, D])
    cv = din("cache_mem_v", [2, 16, 256, D])
    spool = din("state_pool", [16, 15, 512])
    sshift = din("state_shift", [16, 1792])
    swkv = din("state_wkv", [16, 8, 64, 64])
    sconv = din("state_conv", [16, 2, D])
    w_xq = din("w_xq", [2, D, D]); w_xk = din("w_xk", [2, D, D]); w_xv = din("w_xv", [2, D, D]); w_xo = din("w_xo", [2, D, D])
    w_in_ab = din("w_in_ab", [D, 2304])
    pool_w = din("pool_w", [4, 128, 128])
    rw_w2 = din("rw_w2", [64, 512]); rw_a2 = din("rw_a2", [64, 512]); rw_g2 = din("rw_g2", [128, 512])
    w_out_ab = din("w_out_ab", [D, D])
    ffn_gate = din("ffn_gate", [D, 2816]); ffn_up = din("ffn_up", [D, 2816]); ffn_down = din("ffn_down", [2816, D])
    w_in_c = din("w_in_c", [D, 3072]); w_out_c = din("w_out_c", [D, D])
    router_w = din("router_w", [D, 8])
    moe_gate = din("moe_gate", [8, D, 3584]); moe_up = din("moe_up", [8, D, 3584]); moe_down = din("moe_down", [8, 3584, D])
    vecs_d = din("vecs", [128, NVEC])
    consts_d = din("consts", [128, NCONST])
    constsb_d = din("constsb", [128, NCB])

    y_prompt = dout("y_prompt", [2048, D]); y_sample = dout("y_sample", [16, D])
    pool_p = dout("pool_p", [15, 512]); pool_s = dout("pool_s", [16, 15, 512])
    shift_p = dout("shift_p", [14, 128]); shift_s = dout("shift_s", [16, 1792])
    wkv_p = dout("wkv_p", [8, 64, 64]); wkv_s = dout("wkv_s", [16, 8, 64, 64])
    conv_p = dout("conv_p", [2, D]); conv_s = dout("conv_s", [16, 2, D])
    mem_k_p = dout("mem_k_p", [2, 256, D]); mem_v_p = dout("mem_v_p", [2, 256, D])

    def sb(name, shape, dt=F32):
        return st.enter_context(nc.sbuf_tensor(name, list(shape), dt))

    X = sb("X", [128, 8, NT])
    XN = sb("XN", [128, 8, NT], BF16)
    A1 = sb("A1", [128, 8, NT], BF16)
    AR = sb("AR", [128, NSLOT, 512])
    CF = sb("CF", [128, NCONST])
    CB = sb("CB", [128, NCB], BF16)
    VEC = sb("VEC", [128, NVEC])
    SM = sb("SM", [128, 1024])
    FLI = sb("FLI", [1, 2], mybir.dt.int32)
    PS = st.enter_context(nc.psum_tensor("PS", [128, 8, 512], F32))

    identF = CF[:, C_ID:C_ID + 128]
    blkF = CF[:, C_BLK:C_BLK + 128]
    onesF = CF[:, C_ONE:C_ONE + 128]
    EF = CF[:, C_E:C_E + 64]
    EPSC = CF[:, C_EPS:C_EPS + 1]
    EPSL = CF[:, C_EPS + 1:C_EPS + 2]
    identB = CB[:, B_ID:B_ID + 128]
    onesB = CB[:, B_ONE:B_ONE + 128]
    bmB = CB[:, B_BM:B_BM + 128]
    IEB = CB[:, B_IE:B_IE + 192]
    mskB = CB[:, B_MSU:B_MSU + 192]
    mslB = CB[:, B_MSL:B_MSL + 128]
    RSTM = CB[:, B_RST:B_RST + 512]

    def vcol(name, j=0):
        c = VEC_COLS[name] + j
        return VEC[:, c:c + 1]

    def kX(kc, tt): return ("X", kc, tt)
    def kXN(kc, tt): return ("XN", kc, tt)
    def kA1(kc, tt): return ("A1", kc, tt)
    def kS(i, n=1): return [("AR", i + j) for j in range(n)]
    KA1ALL = [kA1(kc, ti) for kc in range(8) for ti in range(5)]
    kC = ("C",)
    kCB = ("CB",)
    kV = ("V",)
    kSM = lambda nm: ("SM", nm)
    kW = lambda nm: ("W", nm)

    def arf(i, n=1):
        return AR[:, i:i + n, :].rearrange("p a b -> p (a b)")

    def arb(i, n=1):
        return AR[:, i:i + n, :].rearrange("p a b -> p (a b)").bitcast(BF16)

    bank_rr = [0]

    def bank():
        b = bank_rr[0]
        bank_rr[0] = (b + 1) % 8
        return b

    def bank2():
        b0 = bank()
        while b0 % 2:
            b0 = bank()
        b1 = bank()
        return b0, b1

    def kP(b): return ("ps", b)

    def mm(out, lhsT, rhs, start, stop, r, w):
        return P.op("pe", lambda E: E.matmul(out, lhsT, rhs, start=start, stop=stop), r, w)

    def act(out, in_, func, r, w, scale=1.0, bias=None, accum=None):
        kw = {}
        if bias is not None:
            kw["bias"] = bias
        if accum is not None:
            kw["accum_out"] = accum
        return P.op("act", lambda E: E.activation(out, in_, func, scale=scale, **kw), r, w)

    def tt_(out, in0, in1, alu, r, w, eng="dve"):
        return P.op(eng, lambda E: E.tensor_tensor(out, in0, in1, alu), r, w)

    def ts_(out, in0, s1, s2, op0, op1, r, w, eng="dve"):
        return P.op(eng, lambda E: E.tensor_scalar(out, in0, s1, s2, op0, op1), r, w)

    def ts1(out, in0, s1, op0, r, w, eng="dve"):
        return P.op(eng, lambda E: E.tensor_single_scalar(out, in0, s1, op0), r, w)

    def stt(out, in0, scalar, in1, op0, op1, r, w):
        return P.op("dve", lambda E: E.scalar_tensor_tensor(out, in0, scalar, in1, op0, op1), r, w)

    def cp(out, in_, r, w, eng="dve"):
        if eng == "act":
            return P.op("act", lambda E: E.copy(out, in_), r, w)
        return P.op(eng, lambda E: E.tensor_copy(out, in_), r, w)

    def red(out, in_, alu, r, w):
        return P.op("dve", lambda E: E.tensor_reduce(out, in_, AX.X, alu), r, w)

    def recip(out, in_, r, w):
        return P.op("dve", lambda E: E.reciprocal(out, in_), r, w)

    def dma(out, in_, r, w, eng="sp"):
        return P.op(eng, lambda E: E.dma_start(out=out, in_=in_), r, w, dma=True)

    def memset(ap, val, w, eng="dve"):
        return P.op(eng, lambda E: E.memset(ap, val), (), w)

    FEN = SM[:, 1016:1024]

    def fence(r, w):
        return P.op("dve", lambda E: E.memset(FEN, 0.0), list(r), list(w) + [kSM("fence")])

    dma(CF[:], consts_d[:, :], [], [kC])
    dma(VEC[:], vecs_d[:, :], [], [kV])
    dma(CB[:], constsb_d[:, :], [], [kCB], eng="pool")
    c0 = VEC_COLS["mu_shift"]
    ts_(VEC[:, VEC_COLS["omu"]:VEC_COLS["omu"] + 14], VEC[:, c0:c0 + 14], -1.0, 1.0, ALU.mult, ALU.add, [kV], [kV])
    c0 = VEC_COLS["rw_ka"]
    ts_(VEC[:, VEC_COLS["omka"]:VEC_COLS["omka"] + 4], VEC[:, c0:c0 + 4], -1.0, 1.0, ALU.mult, ALU.add, [kV], [kV])

    SM_PL, SM_HS, SM_PSS, SM_US, SM_PC = 0, 16, 272, 496, 560
    SM_CT, SM_CS, SM_SCT = 624, 640, 16
    SM_TMP = 272
    SM_T0 = 1000
    memset(SM[:, 0:272], 0.0, [kSM("PL"), kSM("HS")])
    memset(SM[:, SM_PC:SM_PC + 64], 0.0, [kSM("PC")])

    for i in range(16):
        s0 = (i % 2) * 2
        dma(arf(s0, 2), xp[i * 128:(i + 1) * 128, :], [], kS(s0, 2))
        for hf in range(2):
            b = bank()
            for j in range(4):
                kc = hf * 4 + j
                mm(PS[:, b, j * 128:(j + 1) * 128], arf(s0, 2)[:, kc * 128:(kc + 1) * 128], identF, True, True,
                   kS(s0, 2) + [kC], [kP(b)])
            cp(X[:, hf * 4:hf * 4 + 4, i * 128:(i + 1) * 128], PS[:, b, :].rearrange("p (a b) -> p a b", a=4),
               [kP(b)], [kX(hf * 4 + j, i // 4) for j in range(4)], eng=("act" if hf else "dve"))
    dma(arf(5, 2)[0:16, :], xs[:, :], [], kS(5, 2))
    b = bank()
    for kc in range(8):
        mm(PS[:, b, kc * 16:(kc + 1) * 16], arf(5, 2)[0:16, kc * 128:(kc + 1) * 128], identF[0:16, 0:16], True, True,
           kS(5, 2) + [kC], [kP(b)])
    cp(X[:, :, 2048:2064], PS[:, b, 0:128].rearrange("p (a b) -> p a b", a=8), [kP(b)], [kX(kc, 4) for kc in range(8)])

    SQ0, RSTD = 0, 4
    RSTD_L = [4]

    def rms_stats(ti, o, n):
        RSTD = RSTD_L[0]
        sq = arb(SQ0, 4).rearrange("p (a b) -> p a b", a=8)
        for kc in range(8):
            act(sq[:, kc, 0:n], X[:, kc, o:o + n], AF.Square, [kX(kc, ti)], kS(SQ0, 4))
        b = bank()
        for kc in range(8):
            mm(PS[:, b, 0:n], onesB, sq[:, kc, 0:n], kc == 0, kc == 7, kS(SQ0, 4) + [kCB], [kP(b)])
        rs = arf(RSTD)[:, 0:n]
        act(rs, PS[:, b, 0:n], AF.Ln, [kP(b), kC], kS(RSTD), scale=1.0 / D, bias=EPSC)
        act(rs, rs, AF.Exp, kS(RSTD), kS(RSTD), scale=-0.5)
        return rs

    def rmsnorm(gname, hook=None):
        for ti, (o, n) in enumerate(TT):
            rs = rms_stats(ti, o, n)
            if hook is not None:
                hook(ti, o, n, rs)
            for kc in range(8):
                stt(XN[:, kc, o:o + n], X[:, kc, o:o + n], vcol(gname, kc), rs, ALU.mult, ALU.mult,
                    [kX(kc, ti), kV] + kS(RSTD), [kXN(kc, ti)])

    def out_proj(wd):
        for hf in range(2):
            s0 = 21 + hf * 4
            wv = arb(s0, 4).rearrange("p (a b) -> p a b", a=8)
            dma(wv, wd[:, hf * 512:(hf + 1) * 512].rearrange("(kc p) o -> p kc o", p=128), [], kS(s0, 4), eng="pool")
            for ti, (o, n) in enumerate(TT):
                for j in range(4):
                    oc = hf * 4 + j
                    b = bank()
                    for kc in range(8):
                        mm(PS[:, b, 0:n], wv[:, kc, j * 128:(j + 1) * 128], A1[:, kc, o:o + n], kc == 0, kc == 7,
                           kS(s0, 4) + [kA1(kc, ti)], [kP(b)])
                    tt_(X[:, oc, o:o + n], PS[:, b, 0:n], X[:, oc, o:o + n], ALU.add, [kP(b), kX(oc, ti)], [kX(oc, ti)])

    KTs, VBs, MNs = 5, 7, 9

    def memkv(li):
        mn = arb(MNs, 2).rearrange("p (a b) -> p a b", a=8)
        kt = arb(KTs, 2).rearrange("p (a b) -> p a b", a=8)
        vb = arb(VBs, 2).rearrange("p (a b) -> p a b", a=2)
        for mb in range(2):
            s0 = 29 + mb * 2
            stg = arf(s0, 2)
            dma(stg, memp[mb * 128:(mb + 1) * 128, :], [], kS(s0, 2))
            ss = SM[:, SM_T0 + mb:SM_T0 + mb + 1]
            kss = kSM("t%d" % mb)
            tt_(arf(0, 2), stg, stg, ALU.mult, kS(s0, 2), kS(0, 2))
            red(ss, arf(0, 2), ALU.add, kS(0, 2), [kss])
            act(ss, ss, AF.Sqrt, [kss, kC], [kss], scale=1.0 / D, bias=EPSC)
            recip(ss, ss, [kss], [kss])
            ts1(stg, stg, ss, ALU.mult, kS(s0, 2) + [kss], kS(s0, 2))
            for kc in range(8):
                b = bank()
                mm(PS[:, b, 0:128], stg[:, kc * 128:(kc + 1) * 128], identF, True, True, kS(s0, 2) + [kC], [kP(b)])
                ts1(mn[:, kc, mb * 128:(mb + 1) * 128], PS[:, b, 0:128], vcol("norm_mem%d" % li, kc), ALU.mult,
                    [kP(b), kV], kS(MNs, 2))
        for which, wd, od in (((0, w_xk, mem_k_p), (1, w_xv, mem_v_p)) if 'mkproj' not in SKIP else ()):
            for hf in range(2):
                s0 = 21 + hf * 4
                wv = arb(s0, 4).rearrange("p (a b) -> p a b", a=8)
                dma(wv, wd[li, :, hf * 512:(hf + 1) * 512].rearrange("(kc p) o -> p kc o", p=128), [], kS(s0, 4), eng="pool")
                for mb in range(2):
                    b = bank()
                    for kc in range(8):
                        mm(PS[:, b, :], mn[:, kc, mb * 128:(mb + 1) * 128], wv[:, kc, :], kc == 0, kc == 7,
                           kS(MNs, 2) + kS(s0, 4), [kP(b)])
                    so = (which * 4 + hf * 2 + mb) % 4
                    cp(arf(so), PS[:, b, :], [kP(b)], kS(so), eng="act")
                    if 'mkout' not in SKIP:
                        dma(od[li, mb * 128:(mb + 1) * 128, hf * 512:(hf + 1) * 512], arf(so), kS(so), [("out", "memkv")])
                    if which == 1 and 'mkvb' not in SKIP:
                        cp(vb[:, mb, hf * 512:(hf + 1) * 512], arf(so), kS(so), kS(VBs, 2), eng="act")
                if which == 0 and 'mkkt' not in SKIP:
                    for j in range(4):
                        oc = hf * 4 + j
                        b = bank()
                        for kc in range(8):
                            mm(PS[:, b, 0:256], wv[:, kc, j * 128:(j + 1) * 128], mn[:, kc, :], kc == 0, kc == 7,
                               kS(MNs, 2) + kS(s0, 4), [kP(b)])
                        cp(kt[:, oc, :], PS[:, b, 0:256], [kP(b)], kS(KTs, 2))

    def xattn(li):
        rmsnorm("norm_xattn%d" % li)
        if 'memkv' not in SKIP:
            memkv(li)
        kt = arb(KTs, 2).rearrange("p (a b) -> p a b", a=8)
        vb = arb(VBs, 2).rearrange("p (a b) -> p a b", a=2)
        QT = 11
        qt = arb(QT, 5)[:, 0:2 * NT].rearrange("p (a b) -> p a b", a=2)
        QS = 16
        qs = arf(QS)[:, 0:128].rearrange("p (a b) -> p a b", a=8)
        EB, RI = 17, 19
        for h in range(4):
            s0 = 29 + (h % 2) * 2
            wv = arb(s0, 2).rearrange("p (a b) -> p a b", a=8)
            dma(wv, w_xq[li, :, h * 256:(h + 1) * 256].rearrange("(kc p) o -> p kc o", p=128), [], kS(s0, 2), eng="pool")
            for dc in range(2):
                for ti, (o, n) in enumerate(TT):
                    b = bank()
                    for kc in range(8):
                        mm(PS[:, b, 0:n], wv[:, kc, dc * 128:(dc + 1) * 128], XN[:, kc, o:o + n], kc == 0, kc == 7,
                           kS(s0, 2) + [kXN(kc, ti)], [kP(b)])
                    if ti < 4:
                        act(qt[:, dc, o:o + n], PS[:, b, 0:n], AF.Copy, [kP(b)], kS(QT, 5), scale=1.0 / 16.0)
                    else:
                        act(qs[:, 2 * h + dc, :], PS[:, b, 0:n], AF.Copy, [kP(b)], kS(QS), scale=1.0 / 16.0)
            for ti, (o, n) in enumerate(TT[:4] if 'attn' not in SKIP else []):
                es = EB + (ti % 2)
                eb = arb(es).rearrange("p (a b) -> p a b", a=2)
                for mc in range(2):
                    b = bank()
                    for dc in range(2):
                        mm(PS[:, b, 0:n], kt[:, 2 * h + dc, mc * 128:(mc + 1) * 128], qt[:, dc, o:o + n], dc == 0, dc == 1,
                           kS(KTs, 2) + kS(QT, 5), [kP(b)])
                    act(eb[:, mc, 0:n], PS[:, b, 0:n], AF.Exp, [kP(b)], kS(es))
                b = bank()
                for mc in range(2):
                    mm(PS[:, b, 0:n], onesB, eb[:, mc, 0:n], mc == 0, mc == 1, kS(es) + [kCB], [kP(b)])
                ri = arf(RI + (ti % 2))[:, 0:n]
                act(ri, PS[:, b, 0:n], AF.Ln, [kP(b)], kS(RI + (ti % 2)))
                act(ri, ri, AF.Exp, kS(RI + (ti % 2)), kS(RI + (ti % 2)), scale=-1.0)
                for dc in range(2):
                    b = bank()
                    for mc in range(2):
                        mm(PS[:, b, 0:n], vb[:, mc, h * 256 + dc * 128:h * 256 + (dc + 1) * 128], eb[:, mc, 0:n], mc == 0, mc == 1,
                           kS(VBs, 2) + kS(es), [kP(b)])
                    tt_(A1[:, 2 * h + dc, o:o + n], PS[:, b, 0:n], ri, ALU.mult, [kP(b)] + kS(RI + (ti % 2)), [kA1(2 * h + dc, ti)])
        for bsmp in range(16 if 'samp' not in SKIP else 0):
            par = bsmp % 2
            KBk, KBv = 21, 25
            RB = 17 if par == 0 else 11
            TP = 29 if par == 0 else 0
            kb = arf(KBk, 4).rearrange("p (a b) -> p a b", a=2)
            vv = arf(KBv, 4).rearrange("p (a b) -> p a b", a=2)
            dma(kb, ck[li, bsmp].rearrange("(j p) x -> p j x", p=128), [], kS(KBk, 4))
            dma(vv, cv[li, bsmp].rearrange("(j p) x -> p j x", p=128), [], kS(KBv, 4))
            R = arf(RB, 2).rearrange("p (a b) -> p a b", a=8)
            tt_(R, qs[:, :, bsmp:bsmp + 1].broadcast_to([128, 8, 128]), identF.unsqueeze(1).broadcast_to([128, 8, 128]),
                ALU.mult, kS(QS) + [kC], kS(RB, 2))
            b0, b1 = bank2()
            for hf, bb in ((0, b0), (1, b1)):
                mm(PS[:, bb, :], onesF, arf(RB, 2)[:, hf * 512:(hf + 1) * 512], True, True, kS(RB, 2) + [kC], [kP(bb)])
            qbc = PS[:, b0:b0 + 2, :].rearrange("p a b -> p (a b)")
            tp = arf(TP, 4)
            tt_(tp.rearrange("p (j x) -> p j x", j=2), kb, qbc.unsqueeze(1).broadcast_to([128, 2, 1024]), ALU.mult,
                kS(KBk, 4) + [kP(b0), kP(b1)], kS(TP, 4))
            so_ = SM_T0 + 2 if par == 0 else 920
            sc = SM[:, so_:so_ + 8]
            ksc, kee, ks1 = kSM("sc%d" % par), ("EE", par), kSM("s1%d" % par)
            red(sc, tp.rearrange("p (a d) -> p a d", d=256), ALU.add, kS(TP, 4), [ksc])
            ee = arf(33)[:, par * 8:par * 8 + 8]
            act(ee, sc, AF.Exp, [ksc], [kee])
            b = bank()
            mm(PS[:, b, 0:8], onesF, ee, True, True, [kee, kC], [kP(b)])
            s1 = SM[:, so_ + 8:so_ + 12]
            cp(s1, PS[:, b, 0:4], [kP(b)], [ks1])
            tt_(s1, PS[:, b, 4:8], s1, ALU.add, [kP(b), ks1], [ks1])
            recip(s1, s1, [ks1], [ks1])
            b = bank()
            for oc in range(8):
                hh_ = oc // 2
                for j in range(2):
                    mm(PS[:, b, oc:oc + 1], vv[:, j, oc * 128:(oc + 1) * 128], ee[:, j * 4 + hh_:j * 4 + hh_ + 1], j == 0, j == 1,
                       kS(KBv, 4) + [kee], [kP(b)])
            for hh_ in range(4):
                ts1(A1[:, 2 * hh_:2 * hh_ + 2, 2048 + bsmp], PS[:, b, 2 * hh_:2 * hh_ + 2], s1[:, hh_:hh_ + 1], ALU.mult,
                    [kP(b), ks1], [kA1(2 * hh_, 4), kA1(2 * hh_ + 1, 4)])
        out_proj(w_xo[li])

    A1F = A1[:].rearrange("p a b -> p (a b)")

    def ffn_run(wg, wu, wdn, FF, gate_fn=None):
        nfc = FF // 128
        groups = [(f, min(4, nfc - f)) for f in range(0, nfc, 4)]
        for gi, (f0, nf) in enumerate(groups):
            s0 = 14 + (gi % 2) * 8
            wgv = arb(s0, 4).rearrange("p (a b) -> p a b", a=8)
            wuv = arb(s0 + 4, 4).rearrange("p (a b) -> p a b", a=8)
            wdv = A1F[:, (gi % 2) * 4096:(gi % 2 + 1) * 4096].rearrange("p (a b) -> p a b", a=4)
            kwd = [("A1W", gi % 2)]
            dma(wgv[:, :, 0:nf * 128], wg[:, f0 * 128:(f0 + nf) * 128].rearrange("(kc p) f -> p kc f", p=128), [], kS(s0, 4), eng="pool")
            dma(wuv[:, :, 0:nf * 128], wu[:, f0 * 128:(f0 + nf) * 128].rearrange("(kc p) f -> p kc f", p=128), [], kS(s0 + 4, 4), eng="pool")
            dma(wdv[:, 0:nf, :], wdn[f0 * 128:(f0 + nf) * 128, :].rearrange("(fc p) o -> p fc o", p=128), [], kwd, eng="pool")
            for ti, (o, n) in enumerate(TT):
                hs = (ti % 2) * 2
                hh = arb(hs, 2).rearrange("p (a b) -> p a b", a=4)
                hkeys = kS(hs, 2)
                gb = gate_fn(ti, o, n) if gate_fn is not None else None
                for fc in range(nf):
                    bg_ = bank()
                    for kc in range(8):
                        mm(PS[:, bg_, 0:n], wgv[:, kc, fc * 128:(fc + 1) * 128], XN[:, kc, o:o + n], kc == 0, kc == 7,
                           kS(s0, 4) + [kXN(kc, ti)], [kP(bg_)])
                    bu_ = bank()
                    for kc in range(8):
                        mm(PS[:, bu_, 0:n], wuv[:, kc, fc * 128:(fc + 1) * 128], XN[:, kc, o:o + n], kc == 0, kc == 7,
                           kS(s0 + 4, 4) + [kXN(kc, ti)], [kP(bu_)])
                    sgs = 4 + (fc % 2)
                    sg = arf(sgs)[:, 0:n]
                    act(sg, PS[:, bg_, 0:n], AF.Silu, [kP(bg_)], kS(sgs))
                    if gb is None:
                        tt_(hh[:, fc, 0:n], PS[:, bu_, 0:n], sg, ALU.mult, [kP(bu_)] + kS(sgs), hkeys)
                    else:
                        tt_(sg, PS[:, bu_, 0:n], sg, ALU.mult, [kP(bu_)] + kS(sgs), kS(sgs))
                        tt_(hh[:, fc, 0:n], sg, gb[0], ALU.mult, kS(sgs) + gb[1], hkeys)
                for oc in range(8):
                    b = bank()
                    for fc in range(nf):
                        mm(PS[:, b, 0:n], wdv[:, fc, oc * 128:(oc + 1) * 128], hh[:, fc, 0:n], fc == 0, fc == nf - 1,
                           kwd + hkeys, [kP(b)])
                    tt_(X[:, oc, o:o + n], PS[:, b, 0:n], X[:, oc, o:o + n], ALU.add, [kP(b), kX(oc, ti)], [kX(oc, ti)])

    def moe_routed(POSM, GTM):
        MT = [(i * 384, 384, [3 * i, 3 * i + 1, 3 * i + 2]) for i in range(5)] + [(1920, 144, [15, 16])]
        XNF = XN[:].rearrange("p a b -> p (a b)")
        YG = XNF[:, 0:12288].bitcast(F32).rearrange("p (t o) -> p t o", t=6)
        STa = XNF[:, 12288:14592].rearrange("p (t n) -> p t n", t=6)
        HHr = XNF[:, 14592:15616].rearrange("p (b f s) -> p b f s", b=2, f=4)
        SGr = XNF[:, 15616:16128]
        XNT16 = arb(0)
        xgall = arb(2, 6).rearrange("p (k s) -> p k s", k=8)
        XGK = [("XG", t) for t in range(6)]
        hhA = arb(9).rearrange("p (f s) -> p f s", f=2)
        hhB = XNF[:, 14592:15616].rearrange("p (f s) -> p f s", f=2)

        def hhf(fc):
            return hhA[:, fc, :] if fc < 2 else hhB[:, fc - 2, :]
        BT = [(0, 512, [0, 1, 2, 3]), (512, 256, [4, 5])]
        iota = CF[:, C_IOTA:C_IOTA + 128]
        KXNALL = [kXN(kc, ti) for kc in range(8) for ti in range(5)]

        def xnt(blk):
            return A1F[:, blk * 1024:(blk + 1) * 1024] if blk < 16 else XNT16

        def xkeys(oc, o, n):
            return [kX(oc, t) for t in range(5) if TT[t][0] < o + n and TT[t][0] + TT[t][1] > o]
        fence(KA1ALL + kS(0), [("XNT", b_) for b_ in range(17)])
        memset(XNT16, 0.0, [("XNT", 16)])
        for blk in range(17):
            n = 128 if blk < 16 else 16
            o = blk * 128
            b0, b1 = bank2()
            for kc in range(8):
                bb = b0 if kc < 4 else b1
                mm(PS[0:n, bb, (kc % 4) * 128:(kc % 4 + 1) * 128], XN[:, kc, o:o + n], identB, True, True,
                   [kXN(kc, min(4, o // 512)), kCB], [kP(bb)])
            cp(xnt(blk)[0:n, 0:512], PS[0:n, b0, :], [kP(b0)], [("XNT", blk)], eng="act")
            cp(xnt(blk)[0:n, 512:1024], PS[0:n, b1, :], [kP(b1)], [("XNT", blk)])
        fence(KXNALL, [("YG", t, h) for t in range(6) for h in range(2)] + [("ST", t) for t in range(6)] + [("SGR",), ("HHR", 0), ("HHR", 1), ("HHR", 2), ("HHR", 3)] + kS(9))
        for e in range(8):
            for ti, (o, n, blks) in enumerate(MT):
                nb = len(blks)
                b0_ = blks[0]
                ssl = 1 if ti % 2 == 0 else 8
                sel = arb(ssl)[:, 0:nb * 128].rearrange("p (b s) -> p b s", b=nb)
                selg = arb(ssl)[:, 384:384 + nb * 128].rearrange("p (b s) -> p b s", b=nb)
                tt_(sel, iota.unsqueeze(1).broadcast_to([128, nb, 128]),
                    POSM[:, b0_:b0_ + nb, e:e + 1].broadcast_to([128, nb, 128]), ALU.is_equal, [kC, kSM("POSM")], [("SEL", ssl)])
                tt_(selg, sel, GTM[:, b0_:b0_ + nb, e:e + 1].broadcast_to([128, nb, 128]), ALU.mult,
                    [("SEL", ssl), kSM("GTM")], [("SELG", ssl)])
                g0, g1 = bank2()
                for kc in range(8):
                    bb = g0 if kc < 4 else g1
                    for bi, blk in enumerate(blks):
                        nt = 128 if blk < 16 else 16
                        mm(PS[:, bb, (kc % 4) * 128:(kc % 4 + 1) * 128], xnt(blk)[0:nt, kc * 128:(kc + 1) * 128], sel[0:nt, bi, :],
                           bi == 0, bi == nb - 1, [("XNT", blk), ("SEL", ssl)], [kP(bb)])
                cp(xgall[:, 0:4, ti * 128:(ti + 1) * 128], PS[:, g0, :].rearrange("p (k s) -> p k s", k=4), [kP(g0)], [("XG", ti)], eng="act")
                cp(xgall[:, 4:8, ti * 128:(ti + 1) * 128], PS[:, g1, :].rearrange("p (k s) -> p k s", k=4), [kP(g1)], [("XG", ti)])
                b = bank()
                for bi, blk in enumerate(blks):
                    nt = 128 if blk < 16 else 16
                    mm(PS[:, b, bi * 128:bi * 128 + nt], selg[0:nt, bi, :], identB[0:nt, 0:nt], True, True,
                       [("SELG", ssl), kCB], [kP(b)])
                cp(STa[:, ti, 0:n], PS[:, b, 0:n], [kP(b)], [("ST", ti)], eng="act")
            for gi in range(7):
                f0 = gi * 4
                s0 = 10 + (gi % 2) * 12
                wgv = arb(s0, 4).rearrange("p (a b) -> p a b", a=8)
                wuv = arb(s0 + 4, 4).rearrange("p (a b) -> p a b", a=8)
                wdv = arb(s0 + 8, 4).rearrange("p (a b) -> p a b", a=4)
                dma(wgv, moe_gate[e][:, f0 * 128:(f0 + 4) * 128].rearrange("(kc p) f -> p kc f", p=128), [], kS(s0, 4), eng="pool")
                dma(wuv, moe_up[e][:, f0 * 128:(f0 + 4) * 128].rearrange("(kc p) f -> p kc f", p=128), [], kS(s0 + 4, 4), eng="pool")
                dma(wdv, moe_down[e][f0 * 128:(f0 + 4) * 128, :].rearrange("(fc p) o -> p fc o", p=128), [], kS(s0 + 8, 4), eng="pool")
                for (sb0, nn, tis) in BT:
                    xk = [("XG", t) for t in tis]
                    for fc in range(4):
                        bg_, bu_ = bank(), bank()
                        for kc in range(8):
                            mm(PS[:, bg_, 0:nn], wgv[:, kc, fc * 128:(fc + 1) * 128], xgall[:, kc, sb0:sb0 + nn], kc == 0, kc == 7,
                               kS(s0, 4) + xk, [kP(bg_)])
                        for kc in range(8):
                            mm(PS[:, bu_, 0:nn], wuv[:, kc, fc * 128:(fc + 1) * 128], xgall[:, kc, sb0:sb0 + nn], kc == 0, kc == 7,
                               kS(s0 + 4, 4) + xk, [kP(bu_)])
                        act(SGr[:, 0:nn], PS[:, bg_, 0:nn], AF.Silu, [kP(bg_)], [("SGR",)])
                        tt_(hhf(fc)[:, 0:nn], PS[:, bu_, 0:nn], SGr[:, 0:nn], ALU.mult, [kP(bu_), ("SGR",)], [("HHR", fc)])
                    for j, ti in enumerate(tis):
                        for hf in range(2):
                            bd = bank()
                            for fc in range(4):
                                mm(PS[:, bd, :], hhf(fc)[:, j * 128:(j + 1) * 128], wdv[:, fc, hf * 512:(hf + 1) * 512], fc == 0, fc == 3,
                                   [("HHR", fc)] + kS(s0 + 8, 4), [kP(bd)])
                            ygk = [("YG", ti, hf)]
                            if gi == 0:
                                cp(YG[:, ti, hf * 512:(hf + 1) * 512], PS[:, bd, :], [kP(bd)], ygk, eng=("act" if hf else "dve"))
                            else:
                                tt_(YG[:, ti, hf * 512:(hf + 1) * 512], PS[:, bd, :], YG[:, ti, hf * 512:(hf + 1) * 512], ALU.add, [kP(bd)] + ygk, ygk)
            for ti, (o, n, blks) in enumerate(MT):
                ygb = arb(2 + ti)
                cp(ygb, YG[:, ti, :], [("YG", ti, 0), ("YG", ti, 1)], XGK + [("YGB", ti)], eng="act")
                for oc in range(8):
                    b = bank()
                    mm(PS[:, b, 0:n], ygb[:, oc * 128:(oc + 1) * 128], STa[:, ti, 0:n], True, True, XGK + [("YGB", ti), ("ST", ti)], [kP(b)])
                    tt_(X[:, oc, o:o + n], PS[:, b, 0:n], X[:, oc, o:o + n], ALU.add, [kP(b)] + xkeys(oc, o, n), xkeys(oc, o, n))
        fence([("XNT", b_) for b_ in range(17)], KA1ALL)

    def ffn_dense():
        rmsnorm("norm_ffn0")
        fence(KA1ALL, [("A1W", 0), ("A1W", 1)])
        ffn_run(ffn_gate, ffn_up, ffn_down, 2816)
        fence([("A1W", 0), ("A1W", 1)], KA1ALL)

    def moe():
        WR = arf(6)[:, 0:64].rearrange("p (kc e) -> p kc e", kc=8)
        dma(WR, router_w.rearrange("(kc p) e -> p kc e", p=128), [], kS(6))
        for kc in range(8):
            ts1(WR[:, kc, :], WR[:, kc, :], vcol("norm_ffn1", kc), ALU.mult, kS(6) + [kV], kS(6))
        LGT = arf(7, 5)[:, 0:NT]
        SEL = arf(12, 2).rearrange("p (e q) -> p e q", e=8)
        cp(SEL[0:8], identF[0:8, 0:8].unsqueeze(2).broadcast_to([8, 8, 128]), [kC], kS(12, 2))

        def hook(ti, o, n, rs):
            b = bank()
            for kc in range(8):
                mm(PS[0:8, b, 0:n], WR[:, kc, :], X[:, kc, o:o + n], kc == 0, kc == 7, kS(6) + [kX(kc, ti)], [kP(b)])
            tt_(LGT[0:8, o:o + n], PS[0:8, b, 0:n], rs[0:8, :], ALU.mult, [kP(b)] + kS(RSTD), kS(7, 5))
            ts1(LGT[0:8, o:o + n], LGT[0:8, o:o + n], VEC[0:8, VEC_COLS["router_b"]:VEC_COLS["router_b"] + 1], ALU.add,
                kS(7, 5) + [kV], kS(7, 5))
        rmsnorm("norm_ffn1", hook=hook)
        blocks = [(i * 128, 128) for i in range(16)] + [(2048, 16)]
        GTM = SM[:, 336:472].rearrange("p (b e) -> p b e", e=8)
        POSM = SM[:, 768:904].rearrange("p (b e) -> p b e", e=8)
        memset(SM[:, 336:472], 0.0, [kSM("GTM")])
        memset(SM[:, 768:904], -1.0, [kSM("POSM")])
        kt_ = [kSM("top")]
        TS = arf(30, 2)
        v3 = lambda a: a.rearrange("p (b e) -> p b e", e=8)
        lgf, mk1f, l2f, mk2f = TS[:, 0:136], TS[:, 136:272], TS[:, 272:408], TS[:, 408:544]
        m1, m2, w1, w2 = TS[:, 544:561], TS[:, 561:578], TS[:, 578:595], TS[:, 595:612]
        kts = kS(30, 2)
        memset(lgf, 0.0, kts)
        bt = bank()
        for bi_, (o, n) in enumerate(blocks):
            mm(PS[0:n, bt, bi_ * 8:(bi_ + 1) * 8], LGT[0:8, o:o + n], identF[0:8, 0:8], True, True, kS(7, 5) + [kC], [kP(bt)])
        cp(lgf[:, 0:128], PS[:, bt, 0:128], [kP(bt)], kts)
        cp(lgf[0:16, 128:136], PS[0:16, bt, 128:136], [kP(bt)], kts)
        bc3 = lambda a: a.unsqueeze(2).broadcast_to([128, 17, 8])
        red(m1, v3(lgf), ALU.max, kts, kts)
        tt_(v3(mk1f), v3(lgf), bc3(m1), ALU.is_equal, kts, kts)
        stt(l2f, mk1f, -1e30, lgf, ALU.mult, ALU.add, kts, kts)
        red(m2, v3(l2f), ALU.max, kts, kts)
        tt_(v3(mk2f), v3(l2f), bc3(m2), ALU.is_equal, kts, kts)
        tt_(w2, m2, m1, ALU.subtract, kts, kts)
        act(w2, w2, AF.Exp, kts, kts)
        ts1(w1, w2, 1.0, ALU.add, kts, kts)
        recip(w1, w1, kts, kts)
        tt_(w2, w2, w1, ALU.mult, kts, kts)
        tt_(GTM, v3(mk1f), bc3(w1), ALU.mult, kts, [kSM("GTM")])
        tt_(v3(mk2f), v3(mk2f), bc3(w2), ALU.mult, kts, kts)
        tt_(GTM, GTM, v3(mk2f), ALU.add, kts + [kSM("GTM")], [kSM("GTM")])
        for q0 in range(0, 17, 4):
            bq = bank()
            grp = blocks[q0:q0 + 4]
            for j_, (o, n) in enumerate(grp):
                mm(PS[0:8, bq, j_ * 128:j_ * 128 + n], GTM[0:n, q0 + j_, :], identF[0:n, 0:n], True, True, [kSM("GTM"), kC], [kP(bq)])
            o0 = grp[0][0]
            tot = sum(n for (_, n) in grp)
            cp(LGT[0:8, o0:o0 + tot], PS[0:8, bq, 0:tot], [kP(bq)], kS(7, 5), eng="act")
        MK = arf(14, 5)[0:8, 0:NT]; POS = arf(19, 5)[0:8, 0:NT]; RS = arf(24, 5)[0:8, 0:NT]
        ts1(MK, LGT[0:8, :], 0.0, ALU.is_gt, kS(7, 5), kS(14, 5))
        memset(RS, 1.0, kS(24, 5))
        for t0_ in range(0, 2064, 384):
            memset(RS[:, t0_:t0_ + 1], 0.0, kS(24, 5))
        P.op("dve", lambda E: E.tensor_tensor_scan(POS, RS, MK, 0.0, ALU.mult, ALU.add), kS(24, 5) + kS(14, 5), kS(19, 5))
        CN = SM[0:8, SM_T0:SM_T0 + 6]
        cp(CN[:, 0:5], POS[:, 0:1920].rearrange("p (t n) -> p t n", n=384)[:, :, 383], kS(19, 5), [kSM("CN")])
        cp(CN[:, 5:6], POS[:, 2063:2064], kS(19, 5), [kSM("CN")])
        m8 = SM[0:8, SM_T0 + 6:SM_T0 + 7]
        red(m8, CN, ALU.max, [kSM("CN")], [kSM("m8")])
        b = bank()
        mm(PS[0:1, b, 0:8], m8, identF[0:8, 0:8], True, True, [kSM("m8"), kC], [kP(b)])
        mx = SM[0:1, SM_T0 + 8:SM_T0 + 9]
        red(mx, PS[0:1, b, 0:8], ALU.max, [kP(b)], [kSM("mx")])
        ts1(mx, mx, 128.5, ALU.is_gt, [kSM("mx")], [kSM("mx")])
        cp(FLI[0:1, 0:1], mx, [kSM("mx")], [("FLI",)])
        tt_(POS, POS, MK, ALU.mult, kS(19, 5) + kS(14, 5), kS(19, 5))
        ts1(POS, POS, -1.0, ALU.add, kS(19, 5), kS(19, 5))
        for (o, n) in blocks:
            b = bank()
            mm(PS[0:n, b, 0:8], POS[0:8, o:o + n], identF[0:8, 0:8], True, True, kS(19, 5) + [kC], [kP(b)])
            cp(POSM[0:n, o // 128, :], PS[0:n, b, 0:8], [kP(b)], [kSM("POSM")])
        P.flag_ap = FLI[0:1, 0:1]
        P.flagload(["pe", "act", "dve", "pool"], [("FLI",)])
        if 'noroute' in SKIP:
            P.region_begin(1)
            P.region_end()
        P.region_begin(1)
        fence(KA1ALL, [("A1W", 0), ("A1W", 1)])
        gbt = arb(30, 3)
        for e in range(8):
            def gate_fn(ti, o, n, e=e):
                return (gbt[:, ti * 512:ti * 512 + n], [("GB", ti)])
            for ti, (o, n) in enumerate(TT):
                b = bank()
                mm(PS[:, b, 0:n], SEL[0:8, e, :], LGT[0:8, o:o + n], True, True, kS(7, 5) + kS(12, 2), [kP(b)])
                cp(gbt[:, ti * 512:ti * 512 + n], PS[:, b, 0:n], [kP(b)], [("GB", ti)], eng="act")
            ffn_run(moe_gate[e], moe_up[e], moe_down[e], 3584, gate_fn=gate_fn)
        fence([("A1W", 0), ("A1W", 1)], KA1ALL)
        P.region_end()
        P.region_begin(0)
        moe_routed(POSM, GTM)
        P.region_end()

    S_R, S_K, S_V, S_A, S_G, S_LW, S_KK, S_T1, S_T2, S_BON = 0, 1, 2, 3, 28, 29, 30, 31, 32, 33
    S_Y = S_T2
    XNFL = XN[:].rearrange("p a b -> p (a b)")
    XNL = XNFL[:, 0:4096].rearrange("p (a b) -> p a b", a=8)
    RX = XNFL[:, 4096:16512]
    RA = AR[:, 4:12, :].rearrange("p a b -> p (a b)").bitcast(BF16)
    RBg = AR[:, 19:22, :].rearrange("p a b -> p (a b)").bitcast(BF16)
    NC4 = 8

    def carve(reg, off, shp, dt=BF16):
        n = shp[0] * shp[1]
        if dt == F32:
            v = reg[:, off:off + 2 * n].bitcast(F32)
        else:
            v = reg[:, off:off + n]
        return v.rearrange("p (a b) -> p a b", a=shp[0])
    WX1 = carve(RX, 0, [NC4, 320]); WBU = carve(RX, 2560, [NC4, 320]); WNQ = carve(RX, 5120, [NC4, 256])
    WGT = carve(RX, 7168, [NC4, 128], F32); WBR = carve(RX, 9216, [NC4, 192]); WVV = carve(RX, 10752, [NC4, 192])
    WZA = carve(RA, 0, [NC4, 192]); WZK = carve(RA, 1536, [NC4, 192]); WCC = carve(RA, 3072, [NC4, 64], F32)
    WA = carve(RA, 4096, [NC4, 128]); WK = carve(RA, 5120, [NC4, 128]); WV = carve(RA, 6144, [NC4, 128]); WAp = carve(RA, 7168, [NC4, 128])
    WKp = carve(RBg, 0, [NC4, 128]); WNT = carve(RBg, 1024, [NC4, 256])
    WHP = arb(17)[:, 0:256].rearrange("p (a b) -> p a b", a=2)
    WRH = arb(17)[:, 256:768].rearrange("p (a b) -> p a b", a=NC4)
    WNAMES = ["X1", "BU", "NQ", "GT", "BR", "VV", "ZA", "ZK", "CC", "A", "K", "V", "Ap", "Kp", "NT", "HP0", "HP1", "RH"]
    WKEYS = [kW(nm) for nm in WNAMES]
    REGKEYS = kS(4, 8) + kS(19, 3) + kS(17)

    KXNALL0 = [kXN(kc, ti) for kc in range(8) for ti in range(5)]
    kXL = lambda kc: ("XNL", kc)

    def l0_mixer_run():
        fence(KXNALL0, [kXL(kc) for kc in range(8)] + WKEYS)
        PW = arb(12)[:, 0:512].rearrange("p (a b) -> p a b", a=4)
        dma(PW, pool_w.rearrange("g c d -> c g d"), [], kS(12), eng="pool")
        W2A2 = arb(13)[:, 0:512]
        dma(W2A2[0:64, :], rw_w2[:, :], [], kS(13), eng="pool")
        dma(W2A2[64:128, :], rw_a2[:, :], [], kS(13), eng="pool")
        G2 = arb(13)[:, 512:1024]
        dma(G2, rw_g2[:, :], [], kS(13), eng="pool")
        PWK = arf(14, 2)
        PC = SM[:, SM_PC:SM_PC + 64].rearrange("p (g x) -> p g x", g=4)
        PL = SM[:, SM_PL:SM_PL + 14]
        HS = SM[:, SM_HS:SM_HS + 256].rearrange("p (g v) -> p g v", g=4)
        PSS = SM[:, SM_PSS:SM_PSS + 224].rearrange("p (c b) -> p c b", c=14)
        US = SM[:, SM_US:SM_US + 64].rearrange("p (g b) -> p g b", g=4)
        TDA = arb(16)[:, 0:512]
        SGB = arb(16)[:, 512:1024]
        DPOOL = arb(27)[:, 0:512]
        SSH = arf(18)[:, 0:224].rearrange("p (c b) -> p c b", c=14)
        SPT = arf(4, 2)[:, 0:960].rearrange("p (g b r) -> p g b r", g=4, b=16)
        wp_rr = [0]

        def proj(col0, ti, o, n):
            s = 22 + wp_rr[0]
            wp_rr[0] = (wp_rr[0] + 1) % 4
            wv = arb(s).rearrange("p (a b) -> p a b", a=8)
            dma(wv, w_in_ab[:, col0:col0 + 128].rearrange("(kc p) f -> p kc f", p=128), [], kS(s), eng="pool")
            b = bank()
            for kc in range(8):
                mm(PS[:, b, 0:n], wv[:, kc, :], XNL[:, kc, 0:n], kc == 0, kc == 7, kS(s) + [kXL(kc)], [kP(b)])
            return b

        def proj_mix(rc, ti, o, n, dst, dkeys):
            b = proj(512 + rc * 128, ti, o, n)
            pt = arf(26)
            cp(pt[:, 0:n], PS[:, b, 0:n], [kP(b)], kS(26), eng="act")
            tm = arf(27)[:, 0:n]
            if ti < 4:
                ts1(tm[:, 1:n], pt[:, 0:n - 1], vcol("mu_shift", rc), ALU.mult, kS(26) + [kV], kS(27))
                ts1(tm[:, 0:1], PL[:, rc:rc + 1], vcol("mu_shift", rc), ALU.mult, [kSM("PL"), kV], kS(27))
            else:
                ts1(tm, SSH[:, rc, :], vcol("mu_shift", rc), ALU.mult, kS(18) + [kV], kS(27))
                cp(PSS[:, rc, :], pt[:, 0:n], kS(26), [kSM("PSS")])
            stt(dst, pt[:, 0:n], vcol("omu", rc), tm, ALU.mult, ALU.add, kS(26) + kS(27) + [kV], dkeys)
            if ti < 4:
                cp(PL[:, rc:rc + 1], pt[:, n - 1:n], kS(26), [kSM("PL")])

        fence(REGKEYS, WKEYS)
        for ti, (o, n) in enumerate(TT):
            RSTD_L[0] = 26
            rs = rms_stats(ti, o, n)
            for kc in range(8):
                stt(XNL[:, kc, 0:n], X[:, kc, o:o + n], vcol("norm_mix0", kc), rs, ALU.mult, ALU.mult,
                    [kX(kc, ti), kV] + kS(26), [kXL(kc)])
            RSTD_L[0] = 4
            if ti == 4:
                fence(WKEYS, REGKEYS)
                SP = arf(8, 4)
                for hb in range(2):
                    dma(SP[0:120, hb * 512:(hb + 1) * 512], spool[hb * 8:(hb + 1) * 8].rearrange("b r c -> (b r) c"), [], kS(8, 4))
                for hb in range(2):
                    for g in range(4):
                        b = bank()
                        mm(PS[:, b, 0:120], SP[0:120, hb * 512 + g * 128:hb * 512 + (g + 1) * 128], identF[0:120, 0:120], True, True,
                           kS(8, 4) + [kC], [kP(b)])
                        cp(SPT[:, g, hb * 8:(hb + 1) * 8, :], PS[:, b, 0:120].rearrange("p (b r) -> p b r", b=8), [kP(b)], kS(4, 2))
                dma(pool_s[:, 0:14, :], spool[:, 1:15, :], [], [("out", "pool_s")])
                dma(arf(8, 4)[0:16, 0:1792], sshift[:, :], kS(8, 4), kS(8, 4))
                b = bank()
                for rc in range(14):
                    mm(PS[:, b, rc * 16:(rc + 1) * 16], arf(8, 4)[0:16, rc * 128:(rc + 1) * 128], identF[0:16, 0:16], True, True,
                       kS(8, 4) + [kC], [kP(b)])
                cp(SSH, PS[:, b, 0:224].rearrange("p (c b) -> p c b", c=14), [kP(b)], kS(18))
            for g in range(4):
                w = POOL_W[g]
                b = proj(g * 128, ti, o, n)
                if ti < 4:
                    cp(PWK[:, 0:16], PC[:, g, :], [kSM("PC")], kS(14, 2))
                    cp(PWK[:, 16:16 + n], PS[:, b, 0:n], [kP(b)], kS(14, 2), eng="act")
                    cur, curk, lo, step = PWK, kS(14, 2), 0, 1
                    bufs = [(S_T1, arf(S_T1, 2)), (S_LW, arf(S_LW, 2))]
                    bi = 0
                    while step < w:
                        lo2 = lo + step
                        ds, dv = bufs[bi]
                        bi ^= 1
                        tt_(dv[:, lo2:16 + n], cur[:, lo2:16 + n], cur[:, lo2 - step:16 + n - step], ALU.add, curk, kS(ds, 2))
                        cur, curk, lo = dv, kS(ds, 2), lo2
                        step *= 2
                    stt(DPOOL[:, 0:n], cur[:, 16:16 + n], 1.0 / w, PWK[:, 16:16 + n], ALU.mult, ALU.subtract, curk + kS(14, 2), kS(27))
                    if ti == 0:
                        t16 = SM[:, SM_T0:SM_T0 + 16]
                        tt_(t16, cur[:, 16:32], CF[:, C_RC + g * 16:C_RC + (g + 1) * 16], ALU.mult, curk + [kC], [kSM("t16")])
                        tt_(DPOOL[:, 0:16], t16, PWK[:, 16:32], ALU.subtract, [kSM("t16")] + kS(14, 2), kS(27))
                    cp(PC[:, g, :], PWK[:, 512:528], kS(14, 2), [kSM("PC")])
                else:
                    cp(US[:, g, :], PS[:, b, 0:n], [kP(b)], [kSM("US")], eng="act")
                    t16 = SM[:, SM_T0:SM_T0 + 16]
                    red(t16, SPT[:, g, :, 16 - w:15], ALU.add, kS(4, 2), [kSM("t16")])
                    tt_(t16, t16, US[:, g, :], ALU.add, [kSM("t16"), kSM("US")], [kSM("t16")])
                    stt(DPOOL[:, 0:n], t16, 1.0 / w, US[:, g, :], ALU.mult, ALU.subtract, [kSM("t16"), kSM("US")], kS(27))
                b2 = bank()
                mm(PS[:, b2, 0:n], PW[:, g, :], DPOOL[:, 0:n], True, True, kS(12) + kS(27), [kP(b2)])
                act(A1[:, g, o:o + n], PS[:, b2, 0:n], AF.Identity, [kP(b2), kV], [kA1(g, ti)], scale=vcol("pool_scale", g))
            if ti == 3:
                stg = arf(S_T1)
                b = bank()
                for g in range(4):
                    mm(PS[0:15, b, g * 128:(g + 1) * 128], PC[:, g, 1:16], identF, True, True, [kSM("PC"), kC], [kP(b)])
                cp(stg[0:15, :], PS[0:15, b, :], [kP(b)], kS(S_T1))
                dma(pool_p[:, :], stg[0:15, :], kS(S_T1), [("out", "pool_p")])
            m12 = arf(S_T1)[:, 0:n]
            proj_mix(12, ti, o, n, m12, kS(S_T1))
            act(TDA[0:64, 0:n], m12[0:64, :], AF.Tanh, kS(S_T1), kS(16))
            cp(TDA[64:128, 0:n], m12[64:128, :], kS(S_T1), kS(16))
            m13 = arf(S_T2)[:, 0:n]
            proj_mix(13, ti, o, n, m13, kS(S_T2))
            act(SGB[:, 0:n], m13, AF.Sigmoid, kS(S_T2), kS(16))
            for g in range(4):
                r_ = arf(S_R)[:, 0:n]; k_ = arf(S_K)[:, 0:n]; v_ = arf(S_V)[:, 0:n]
                a_ = arf(S_A)[:, 0:n]; g_ = arf(S_G)[:, 0:n]; lw = arf(S_LW)[:, 0:n]; kk = arf(S_KK)[:, 0:n]
                t1 = arf(S_T1)[:, 0:n]; t2 = arf(S_T2)[:, 0:n]; bon = arf(S_BON)[:, 0:n]; y_ = arf(S_Y)[:, 0:n]
                K = dict(R=kS(S_R), K=kS(S_K), V=kS(S_V), A=kS(S_A), LW=kS(S_LW), KK=kS(S_KK), T1=kS(S_T1), T2=kS(S_T2), Y=kS(S_Y))
                proj_mix(g, ti, o, n, r_, kS(S_R))
                proj_mix(4 + g, ti, o, n, k_, kS(S_K))
                proj_mix(8 + g, ti, o, n, v_, kS(S_V))
                b = bank()
                mm(PS[:, b, 0:n], W2A2[0:64, g * 128:(g + 1) * 128], TDA[0:64, 0:n], True, True, kS(13) + kS(16), [kP(b)])
                act(lw, PS[:, b, 0:n], AF.Sigmoid, [kP(b), kV], kS(S_LW), bias=vcol("rw_w0", g))
                ts1(lw, lw, -0.6065306597126334, ALU.mult, kS(S_LW), kS(S_LW))
                b = bank()
                mm(PS[:, b, 0:n], W2A2[64:128, g * 128:(g + 1) * 128], TDA[64:128, 0:n], True, True, kS(13) + kS(16), [kP(b)])
                act(a_, PS[:, b, 0:n], AF.Sigmoid, [kP(b), kV], kS(S_A), bias=vcol("rw_a0", g))
                b = bank()
                mm(PS[:, b, 0:n], G2[:, g * 128:(g + 1) * 128], SGB[:, 0:n], True, True, kS(13) + kS(16), [kP(b)])
                cp(g_, PS[:, b, 0:n], [kP(b)], kS(S_G), eng="act")
                ts1(kk, k_, vcol("rw_kk", g), ALU.mult, kS(S_K) + [kV], kS(S_KK))
                tt_(t1, kk, kk, ALU.mult, kS(S_KK), kS(S_T1))
                b = bank()
                mm(PS[:, b, 0:n], blkF, t1, True, True, kS(S_T1) + [kC], [kP(b)])
                ts1(t1, PS[:, b, 0:n], 1e-24, ALU.max, [kP(b)], kS(S_T1))
                act(t1, t1, AF.Ln, kS(S_T1), kS(S_T1))
                act(t1, t1, AF.Exp, kS(S_T1), kS(S_T1), scale=-0.5)
                tt_(kk, kk, t1, ALU.mult, kS(S_KK) + kS(S_T1), kS(S_KK))
                ts1(t1, a_, vcol("rw_ka", g), ALU.mult, kS(S_A) + [kV], kS(S_T1))
                ts1(t1, t1, vcol("omka", g), ALU.add, kS(S_T1) + [kV], kS(S_T1))
                tt_(k_, k_, t1, ALU.mult, kS(S_K) + kS(S_T1), kS(S_K))
                tt_(t1, r_, k_, ALU.mult, kS(S_R) + kS(S_K), kS(S_T1))
                ts1(t1, t1, vcol("rw_rk", g), ALU.mult, kS(S_T1) + [kV], kS(S_T1))
                b = bank()
                mm(PS[:, b, 0:n], blkF, t1, True, True, kS(S_T1) + [kC], [kP(b)])
                tt_(bon, PS[:, b, 0:n], v_, ALU.mult, [kP(b)] + kS(S_V), kS(S_BON))
                stt(a_, kk, -1.0, a_, ALU.mult, ALU.mult, kS(S_KK) + kS(S_A), kS(S_A))
                if ti < 4:
                    wkv_chunked(g, 0, r_, k_, v_, a_, lw, kk, t1, t2, y_, HS, K)
                else:
                    wkv_sample(g, r_, k_, v_, a_, lw, kk, y_, K)
                b = bank()
                mm(PS[:, b, 0:n], blkF, y_, True, True, kS(S_Y) + [kC], [kP(b)])
                stt(y_, PS[:, b, 0:n], -1.0 / 64.0, y_, ALU.mult, ALU.add, [kP(b)] + kS(S_Y), kS(S_Y))
                tt_(t1, y_, y_, ALU.mult, kS(S_Y), kS(S_T1))
                b = bank()
                mm(PS[:, b, 0:n], blkF, t1, True, True, kS(S_T1) + [kC], [kP(b)])
                act(t1, PS[:, b, 0:n], AF.Ln, [kP(b), kC], kS(S_T1), scale=1.0 / 64.0, bias=EPSL)
                act(t1, t1, AF.Exp, kS(S_T1), kS(S_T1), scale=-0.5)
                tt_(y_, y_, t1, ALU.mult, kS(S_Y) + kS(S_T1), kS(S_Y))
                ts1(y_, y_, vcol("rw_lnx_w", g), ALU.mult, kS(S_Y) + [kV], kS(S_Y))
                ts1(y_, y_, vcol("rw_lnx_b", g), ALU.add, kS(S_Y) + [kV], kS(S_Y))
                tt_(y_, y_, bon, ALU.add, kS(S_Y) + kS(S_BON), kS(S_Y))
                tt_(A1[:, 4 + g, o:o + n], y_, g_, ALU.mult, kS(S_Y) + kS(S_G), [kA1(4 + g, ti)])
        stg2 = arf(S_T2)
        b = bank()
        for g in range(4):
            mm(PS[0:16, b, g * 128:(g + 1) * 128], US[:, g, :], identF, True, True, [kSM("US"), kC], [kP(b)])
        cp(stg2[0:16, :], PS[0:16, b, :], [kP(b)], kS(S_T2))
        dma(pool_s[:, 14, :], stg2[0:16, :], kS(S_T2), [("out", "pool_s2")])
        b = bank()
        mm(PS[0:14, b, 0:128], PL, identF, True, True, [kSM("PL"), kC], [kP(b)])
        sp_ = arf(S_R)
        cp(sp_[0:14, 0:128], PS[0:14, b, 0:128], [kP(b)], kS(S_R))
        dma(shift_p[:, :], sp_[0:14, 0:128], kS(S_R), [("out", "shift_p")])
        ss_ = arf(28, 4)
        for grp in range(4):
            b = bank()
            rcs = list(range(grp * 4, min(14, grp * 4 + 4)))
            for j, rc in enumerate(rcs):
                mm(PS[0:16, b, j * 128:(j + 1) * 128], PSS[:, rc, :], identF, True, True, [kSM("PSS"), kC], [kP(b)])
            cp(ss_[0:16, grp * 512:grp * 512 + len(rcs) * 128], PS[0:16, b, 0:len(rcs) * 128], [kP(b)], kS(28, 4))
        dma(shift_s[:, :], ss_[0:16, 0:1792], kS(28, 4), [("out", "shift_s")])
        wst = arf(1, 2)[:, 0:512].rearrange("p (h k) -> p h k", h=8)
        b = bank()
        for g in range(4):
            mm(PS[0:64, b, g * 128:(g + 1) * 128], HS[:, g, :], identF, True, True, [kSM("HS"), kC], [kP(b)])
        cp(wst[0:64, :, :], PS[0:64, b, :].rearrange("p (h k) -> p h k", h=8), [kP(b)], kS(1, 2))
        dma(wkv_p.rearrange("h v k -> v h k"), wst[0:64, :, :], kS(1, 2), [("out", "wkv_p")])
        fence(WKEYS + [kSM("PSS"), kSM("US"), kSM("PC")] + [kXL(kc) for kc in range(8)], REGKEYS + [kSM("top")] + KXNALL0)
        out_proj(w_out_ab)

    def wkv_chunked(g, c0, r_, k_, v_, nkka, lw, kk, t1f, t2f, y_, HS, K):
        NCH = NC4
        sl = slice(c0, c0 + 64 * NCH)
        r_, k_, v_, nkka, lw, kk, t1, t2 = r_[:, sl], k_[:, sl], v_[:, sl], nkka[:, sl], lw[:, sl], kk[:, sl], t1f[:, sl], t2f[:, sl]
        c3 = lambda ap: ap.rearrange("p (c t) -> p c t", t=64)
        P.op("dve", lambda E: E.tensor_tensor_scan(t2, RSTM[:, 0:64 * NCH], lw, 0.0, ALU.mult, ALU.add), K["LW"] + [kCB], K["T2"])
        tt_(t1, t2, lw, ALU.subtract, K["T2"] + K["LW"], K["T1"])
        act(t1, t1, AF.Exp, K["T1"], K["T1"])
        tt_(t1, t1, kk, ALU.mult, K["T1"] + K["KK"], K["T1"])
        bm4 = bmB.rearrange("p (h t) -> p h t", h=2).unsqueeze(1).broadcast_to([128, NCH, 2, 64])

        def to_bp(dst3, src, skeys, dkey):
            tt_(dst3.rearrange("p c (h t) -> p c h t", h=2), c3(src).unsqueeze(2).broadcast_to([128, NCH, 2, 64]), bm4, ALU.mult,
                skeys + [kCB], [dkey])
        to_bp(WBR[:, :, 0:128], t1, K["T1"], kW("BR"))
        act(t1, t2, AF.Exp, K["T2"], K["T1"])
        tt_(WBR[:, :, 128:192], c3(r_), c3(t1), ALU.mult, K["R"] + K["T1"], [kW("BR")])
        plc = SM[:, 912:912 + NCH]
        cp(plc, c3(t1)[:, :, 63], K["T1"], [kSM("plc")])
        act(t2, t2, AF.Exp, K["T2"], K["T2"], scale=-1.0)
        tt_(t1, nkka, t2, ALU.mult, K["A"] + K["T2"], K["T1"])
        to_bp(WA, t1, K["T1"], kW("A"))
        tt_(t1, k_, t2, ALU.mult, K["K"] + K["T2"], K["T1"])
        to_bp(WK, t1, K["T1"], kW("K"))
        to_bp(WV, v_, K["V"], kW("V"))
        ev = [0]

        def ee_():
            ev[0] ^= 1
            return "act" if ev[0] else "dve"
        for c in range(NCH):
            b = bank()
            mm(PS[:, b, 0:128], WA[:, c, :], identB, True, True, [kW("A"), kCB], [kP(b)])
            cp(WAp[:, c, :], PS[:, b, 0:128], [kP(b)], [kW("Ap")], eng=ee_())
            b = bank()
            mm(PS[:, b, 0:128], WK[:, c, :], identB, True, True, [kW("K"), kCB], [kP(b)])
            cp(WKp[:, c, :], PS[:, b, 0:128], [kP(b)], [kW("Kp")], eng=ee_())
            b = bank()
            mm(PS[:, b, 0:128], WBR[:, c, 0:128], identB, True, True, [kW("BR"), kCB], [kP(b)])
            cp(WX1[:, c, 0:128], PS[:, b, 0:128], [kP(b)], [kW("X1")], eng=ee_())
            b = bank()
            mm(PS[:, b, 0:192], WV[:, c, :], IEB, True, True, [kW("V"), kCB], [kP(b)])
            cp(WVV[:, c, :], PS[:, b, 0:192], [kP(b)], [kW("VV")], eng=ee_())
        for c in range(NCH):
            b = bank()
            mm(PS[:, b, 0:192], WA[:, c, :], WBR[:, c, :], True, True, [kW("A"), kW("BR")], [kP(b)])
            tt_(WZA[:, c, :], PS[:, b, 0:192], mskB, ALU.mult, [kP(b), kCB], [kW("ZA")])
            b = bank()
            mm(PS[:, b, 0:192], WK[:, c, :], WBR[:, c, :], True, True, [kW("K"), kW("BR")], [kP(b)])
            tt_(WZK[:, c, :], PS[:, b, 0:192], mskB, ALU.mult, [kP(b), kCB], [kW("ZK")])
            b = bank()
            mm(PS[:, b, 0:128], WBR[:, c, 0:128], WA[:, c, :], True, True, [kW("A"), kW("BR")], [kP(b)])
            tt_(WNT[:, c, 0:128], PS[:, b, 0:128], mslB, ALU.mult, [kP(b), kCB], [kW("NT")])
        for c in range(NCH):
            cp(WNQ[:, c, 0:128], WZA[:, c, 0:128], [kW("ZA")], [kW("NQ")], eng="act")
            tt_(WNQ[:, c, 128:256], WZA[:, c, 0:128], identB, ALU.add, [kW("ZA"), kCB], [kW("NQ")])
            P.op("pool", lambda E, c=c: E.tensor_tensor(WNT[:, c, 128:256], WNT[:, c, 0:128], identB, ALU.add), [kW("NT"), kCB], [kW("NT")])
        for t in range(1, 7):
            for c in range(NCH):
                NTj = WNT[:, c, 0:128]; NTIj = WNT[:, c, 128:256]; Nj = WNQ[:, c, 0:128]; Qj = WNQ[:, c, 128:256]
                if t <= 5:
                    b = bank()
                    if t == 1:
                        mm(PS[:, b, 0:128], NTj, Nj, True, True, [kW("NT"), kW("NQ")], [kP(b)])
                    elif t < 5:
                        mm(PS[:, b, 0:128], NTj, Nj, True, True, [kW("NT"), kW("NQ")], [kP(b)])
                        mm(PS[:, b, 128:256], NTIj, Qj, True, True, [kW("NT"), kW("NQ")], [kP(b)])
                    else:
                        mm(PS[:, b, 128:256], NTIj, Qj, True, True, [kW("NT"), kW("NQ")], [kP(b)])
                    b2 = bank()
                    mm(PS[:, b2, 0:128], Nj, NTj, True, True, [kW("NT"), kW("NQ")], [kP(b2)])
                    mm(PS[:, b2, 128:256], Nj, NTj, True, False, [kW("NT"), kW("NQ")], [kP(b2)])
                    mm(PS[:, b2, 128:256], identB, identB, False, True, [kCB], [kP(b2)])
                    if t == 1:
                        cp(WNQ[:, c, 0:128], PS[:, b, 0:128], [kP(b)], [kW("NQ")], eng="act")
                    elif t < 5:
                        cp(WNQ[:, c, :], PS[:, b, 0:256], [kP(b)], [kW("NQ")], eng="act")
                    else:
                        cp(WNQ[:, c, 128:256], PS[:, b, 128:256], [kP(b)], [kW("NQ")], eng="act")
                    cp(WNT[:, c, :], PS[:, b2, 0:256], [kP(b2)], [kW("NT")], eng="dve")
                else:
                    b = bank()
                    mm(PS[:, b, 0:128], NTIj, Qj, True, True, [kW("NT"), kW("NQ")], [kP(b)])
                    cp(WNQ[:, c, 128:256], PS[:, b, 0:128], [kP(b)], [kW("NQ")], eng=ee_())
        for c in range(NCH):
            b = bank()
            mm(PS[:, b, 0:192], WZK[:, c, 0:128], WVV[:, c, :], True, True, [kW("ZK"), kW("VV")], [kP(b)])
            cp(WX1[:, c, 128:320], PS[:, b, 0:192], [kP(b)], [kW("X1")], eng=ee_())
        for c in range(NCH):
            b = bank()
            mm(PS[:, b, 0:320], WNQ[:, c, 128:256], WX1[:, c, :], True, True, [kW("NQ"), kW("X1")], [kP(b)])
            cp(WBU[:, c, :], PS[:, b, 0:320], [kP(b)], [kW("BU")], eng=ee_())
        for c in range(NCH):
            b = bank()
            mm(PS[:, b, 0:128], WBU[:, c, 0:128], WAp[:, c, :], True, True, [kW("BU"), kW("Ap")], [kP(b)])
            tt_(WGT[:, c, :], PS[:, b, 0:128], identF, ALU.add, [kP(b), kC], [kW("GT")])
            b = bank()
            mm(PS[:, b, 0:64], WAp[:, c, :], WBU[:, c, 256:320], True, False, [kW("BU"), kW("Ap")], [kP(b)])
            mm(PS[:, b, 0:64], WKp[:, c, :], WVV[:, c, 128:192], False, True, [kW("Kp"), kW("VV")], [kP(b)])
            ts1(WCC[:, c, :], PS[:, b, 0:64], plc[:, c:c + 1], ALU.mult, [kP(b), kSM("plc")], [kW("CC")])
            b = bank()
            mm(PS[:, b, 0:64], WBU[:, c, 0:128], WZA[:, c, 128:192], True, True, [kW("BU"), kW("ZA")], [kP(b)])
            tt_(WRH[:, c, :], PS[:, b, 0:64], WBR[:, c, 128:192], ALU.add, [kP(b), kW("BR")], [kW("RH")])
        for c in range(NCH):
            hp = WHP[:, c % 2, :]
            tt_(hp.rearrange("p (h v) -> p h v", h=2), HS[:, g, :].unsqueeze(1).broadcast_to([128, 2, 64]),
                bmB.rearrange("p (h t) -> p h t", h=2), ALU.mult, [kSM("HS"), kCB], [kW("HP%d" % (c % 2))])
            b = bank()
            mm(PS[:, b, 0:64], WBU[:, c, 128:256], WZA[:, c, 128:192], True, False, [kW("BU"), kW("ZA")], [kP(b)])
            mm(PS[:, b, 0:64], WVV[:, c, 0:128], WZK[:, c, 128:192], False, False, [kW("VV"), kW("ZK")], [kP(b)])
            mm(PS[:, b, 0:64], hp, WRH[:, c, :], False, True, [kW("HP%d" % (c % 2)), kW("RH")], [kP(b)])
            cp(y_[:, c0 + c * 64:c0 + (c + 1) * 64], PS[:, b, 0:64], [kP(b)], K["Y"], eng="act")
            b = bank()
            mm(PS[:, b, 0:64], WGT[:, c, :], HS[:, g, :], True, True, [kW("GT"), kSM("HS")], [kP(b)])
            stt(HS[:, g, :], PS[:, b, 0:64], plc[:, c:c + 1], WCC[:, c, :], ALU.mult, ALU.add,
                [kP(b), kSM("plc"), kW("CC")], [kSM("HS")])

    def wkv_sample(g, r_, k_, v_, nkka, lw, kk, y_, K):
        SW = arf(4, 2).rearrange("p (b k) -> p b k", b=16)
        T1 = arf(6, 2).rearrange("p (b k) -> p b k", b=16)
        T2 = arf(8, 2).rearrange("p (b k) -> p b k", b=16)
        RQ = arf(10, 2).rearrange("p (b k) -> p b k", b=16)
        kSW, kT1, kT2, kRQ = kS(4, 2), kS(6, 2), kS(8, 2), kS(10, 2)
        for hh_ in range(2):
            dma(SW[hh_ * 64:(hh_ + 1) * 64], swkv[:, 2 * g + hh_, :, :].rearrange("b v k -> v b k"), [], kSW)
        act(lw, lw, AF.Exp, K["LW"], K["LW"])
        I2 = EF.unsqueeze(1).broadcast_to([128, 16, 64])

        def bcast(q, qk):
            tt_(RQ, q.unsqueeze(2).broadcast_to([128, 16, 64]), I2, ALU.mult, qk + [kC], kRQ)
            b0, b1 = bank2()
            for hf, bb in ((0, b0), (1, b1)):
                mm(PS[:, bb, :], blkF, arf(10, 2)[:, hf * 512:(hf + 1) * 512], True, True, kRQ + [kC], [kP(bb)])
            return PS[:, b0:b0 + 2, :].rearrange("p a (b k) -> p (a b) k", k=64), [kP(b0), kP(b1)]
        skk = SM[:, SM_T0:SM_T0 + 16]
        bc, bk = bcast(kk, K["KK"])
        tt_(T1, SW, bc, ALU.mult, kSW + bk, kT1)
        red(skk, T1, ALU.add, kT1, [kSM("skk")])
        bc, bk = bcast(lw, K["LW"])
        tt_(T2, SW, bc, ALU.mult, kSW + bk, kT2)
        bc, bk = bcast(nkka, K["A"])
        tt_(T1, bc, skk.unsqueeze(2).broadcast_to([128, 16, 64]), ALU.mult, bk + [kSM("skk")], kT1)
        tt_(T2, T2, T1, ALU.add, kT2 + kT1, kT2)
        bc, bk = bcast(k_, K["K"])
        tt_(T1, bc, v_.unsqueeze(2).broadcast_to([128, 16, 64]), ALU.mult, bk + K["V"], kT1)
        tt_(T2, T2, T1, ALU.add, kT2 + kT1, kT2)
        bc, bk = bcast(r_, K["R"])
        tt_(T1, T2, bc, ALU.mult, kT2 + bk, kT1)
        red(y_, T1, ALU.add, kT1, K["Y"])
        for hh_ in range(2):
            dma(wkv_s[:, 2 * g + hh_, :, :].rearrange("b v k -> v b k"), T2[hh_ * 64:(hh_ + 1) * 64], kT2, [("out", "wkv_s", g, hh_)])

    def l1_mixer():
        rmsnorm("norm_mix1")
        SCT = SM[:, SM_SCT:SM_SCT + 256].rearrange("p (j b r) -> p j b r", j=8, b=16)
        CTc = SM[:, SM_CT:SM_CT + 16].rearrange("p (j r) -> p j r", j=8)
        CSs = SM[:, SM_CS:SM_CS + 128].rearrange("p (j b) -> p j b", j=8)
        stg = arf(5, 2)
        dma(stg[0:32, 0:1024], sconv.rearrange("b r c -> (b r) c"), [], kS(5, 2))
        b = bank()
        for j in range(8):
            mm(PS[:, b, j * 32:(j + 1) * 32], stg[0:32, j * 128:(j + 1) * 128], identF[0:32, 0:32], True, True, kS(5, 2) + [kC], [kP(b)])
        cp(SM[:, SM_SCT:SM_SCT + 256], PS[:, b, 0:256], [kP(b)], [kSM("SCT")])
        dma(conv_s[:, 0, :], sconv[:, 1, :], [], [("out", "conv_s0")])
        CT = arf(7, 2)
        BG, CG, ZZ = 9, 10, 11
        for j in range(8):
            s0 = 12 + (j % 2) * 3
            wv = arb(s0, 3).rearrange("p (w kc f) -> p w kc f", w=3, kc=8)
            ks = kS(s0, 3)
            for wi in range(3):
                dma(wv[:, wi], w_in_c[:, wi * 1024 + j * 128:wi * 1024 + (j + 1) * 128].rearrange("(kc p) f -> p kc f", p=128), [], ks, eng="pool")
            memset(CT[:, 0:2], 0.0, kS(7, 2))
            for ti, (o, n) in enumerate(TT):
                bs = []
                for wi in range(3):
                    b = bank()
                    for kc in range(8):
                        mm(PS[:, b, 0:n], wv[:, wi, kc, :], XN[:, kc, o:o + n], kc == 0, kc == 7, ks + [kXN(kc, ti)], [kP(b)])
                    bs.append(b)
                bg = arf(BG)[:, 0:n]; cg = arf(CG)[:, 0:n]; zz = arf(ZZ)[:, 0:n]
                cp(bg, PS[:, bs[0], 0:n], [kP(bs[0])], kS(BG), eng="act")
                cp(cg, PS[:, bs[1], 0:n], [kP(bs[1])], kS(CG), eng="act")
                if ti < 4:
                    tt_(CT[:, 2:2 + n], PS[:, bs[2], 0:n], cg, ALU.mult, [kP(bs[2])] + kS(CG), kS(7, 2))
                    ts1(zz, CT[:, 0:n], vcol("conv_w0", j), ALU.mult, kS(7, 2) + [kV], kS(ZZ))
                    stt(zz, CT[:, 1:1 + n], vcol("conv_w1", j), zz, ALU.mult, ALU.add, kS(7, 2) + kS(ZZ) + [kV], kS(ZZ))
                    stt(zz, CT[:, 2:2 + n], vcol("conv_w2", j), zz, ALU.mult, ALU.add, kS(7, 2) + kS(ZZ) + [kV], kS(ZZ))
                    tt_(A1[:, j, o:o + n], bg, zz, ALU.mult, kS(BG) + kS(ZZ), [kA1(j, ti)])
                    cp(SM[:, SM_T0 + 14:SM_T0 + 16], CT[:, n:n + 2], kS(7, 2), [kSM("ctt")])
                    cp(CT[:, 0:2], SM[:, SM_T0 + 14:SM_T0 + 16], [kSM("ctt")], kS(7, 2))
                    if ti == 3:
                        cp(CTc[:, j, :], CT[:, 0:2], kS(7, 2), [kSM("CT")])
                else:
                    tt_(CSs[:, j, :], PS[:, bs[2], 0:n], cg, ALU.mult, [kP(bs[2])] + kS(CG), [kSM("CS")])
                    ts1(zz, SCT[:, j, :, 0], vcol("conv_w0", j), ALU.mult, [kSM("SCT"), kV], kS(ZZ))
                    stt(zz, SCT[:, j, :, 1], vcol("conv_w1", j), zz, ALU.mult, ALU.add, [kSM("SCT"), kV] + kS(ZZ), kS(ZZ))
                    stt(zz, CSs[:, j, :], vcol("conv_w2", j), zz, ALU.mult, ALU.add, [kSM("CS"), kV] + kS(ZZ), kS(ZZ))
                    tt_(A1[:, j, o:o + n], bg, zz, ALU.mult, kS(BG) + kS(ZZ), [kA1(j, ti)])
        for hf in range(2):
            b = bank()
            for jj in range(4):
                mm(PS[0:2, b, jj * 128:(jj + 1) * 128], CTc[:, hf * 4 + jj, :], identF, True, True, [kSM("CT"), kC], [kP(b)])
            cp(arf(18 + hf)[0:2, :], PS[0:2, b, :], [kP(b)], kS(18 + hf))
            dma(conv_p[:, hf * 512:(hf + 1) * 512], arf(18 + hf)[0:2, :], kS(18 + hf), [("out", "conv_p", hf)])
            b = bank()
            for jj in range(4):
                mm(PS[0:16, b, jj * 128:(jj + 1) * 128], CSs[:, hf * 4 + jj, :], identF, True, True, [kSM("CS"), kC], [kP(b)])
            cp(arf(29 + hf)[0:16, :], PS[0:16, b, :], [kP(b)], kS(29 + hf))
            dma(conv_s[:, 1, hf * 512:(hf + 1) * 512], arf(29 + hf)[0:16, :], kS(29 + hf), [("out", "conv_s1", hf)])
        out_proj(w_out_c)

    def final():
        YF = arf(8, 8).rearrange("p (a b) -> p a b", a=8)
        for ti, (o, n) in enumerate(TT):
            rs = rms_stats(ti, o, n)
            for kc in range(8):
                stt(YF[:, kc, 0:n], X[:, kc, o:o + n], vcol("norm_final", kc), rs, ALU.mult, ALU.mult,
                    [kX(kc, ti), kV] + kS(RSTD), kS(8, 8))
            nb = (n + 127) // 128
            for bi in range(nb):
                m = min(128, n - bi * 128)
                so = 16 + (bi % 2) * 2
                og = arf(so, 2)
                for hf in range(2):
                    b = bank()
                    for j in range(4):
                        kc = hf * 4 + j
                        mm(PS[0:m, b, j * 128:(j + 1) * 128], YF[:, kc, bi * 128:bi * 128 + m], identF, True, True,
                           kS(8, 8) + [kC], [kP(b)])
                    cp(og[0:m, hf * 512:(hf + 1) * 512], PS[0:m, b, :], [kP(b)], kS(so, 2), eng=("act" if hf else "dve"))
                if ti < 4:
                    dma(y_prompt[o + bi * 128:o + bi * 128 + m, :], og[0:m, :], kS(so, 2), [("out", "y", ti, bi)])
                else:
                    dma(y_sample[0:16, :], og[0:16, :], kS(so, 2), [("out", "ys")])

    l0_mixer_run()
    if dbg_stage >= 2:
        xattn(0)
    if dbg_stage >= 3:
        ffn_dense()
    if dbg_stage >= 4:
        l1_mixer()
    if dbg_stage >= 5:
        xattn(1)
    if dbg_stage >= 6:
        moe()
    final()
    P.emit()
    st.close()
    nc._used_inputs = list(USED)
    nc._prog_stats = (P.sig_counts, P.n_ops, list(P.dma_cnt))
    return nc


_CACHE = {}


def _get_nc(dbg_stage=99):
    if dbg_stage not in _CACHE:
        nc = bass.Bass("TRN2", target_bir_lowering=False)
        build_program(nc, dbg_stage)
        _CACHE[dbg_stage] = nc
    return _CACHE[dbg_stage]


def kernel(**inp):
    dbg_stage = int(inp.pop("_dbg_stage", 99))
    f = lambda a: np.ascontiguousarray(np.asarray(a, dtype=np.float32))
    nc = _get_nc(dbg_stage)
    vecs = _pack_vecs(inp)
    consts, constsb = _consts()
    shared = dict(
        w_xq=f(inp["w_xq"]), w_xk=f(inp["w_xk"]), w_xv=f(inp["w_xv"]), w_xo=f(inp["w_xo"]),
        w_in_ab=f(inp["w_in_ab"][0]), pool_w=f(inp["pool_w"][0]),
        rw_w2=f(inp["rw_w2"][0]), rw_a2=f(inp["rw_a2"][0]), rw_g2=f(inp["rw_g2"][0]),
        w_out_ab=f(inp["w_out_ab"][0]),
        ffn_gate=f(inp["ffn_gate"][0]), ffn_up=f(inp["ffn_up"][0]), ffn_down=f(inp["ffn_down"][0]),
        w_in_c=f(inp["w_in_c"][0]), w_out_c=f(inp["w_out_c"][0]), router_w=f(inp["router_w"][0]),
        moe_gate=f(inp["moe_gate"][0]), moe_up=f(inp["moe_up"][0]), moe_down=f(inp["moe_down"][0]),
        vecs=vecs, consts=consts, constsb=constsb,
    )
    in_maps = []
    for c in range(8):
        s = slice(16 * c, 16 * (c + 1))
        m = dict(shared)
        m.update(
            x_prompt=f(inp["x_prompt"][c]), x_sample=f(inp["x_sample"][s, 0]), mem_prompt=f(inp["mem_prompt"][c]),
            cache_mem_k=f(inp["cache_mem_k"][:, s].reshape(2, 16, 256, 1024)),
            cache_mem_v=f(inp["cache_mem_v"][:, s].reshape(2, 16, 256, 1024)),
            state_pool=f(inp["state_pool"][0, s]), state_shift=f(inp["state_shift"][0, s]),
            state_wkv=f(inp["state_wkv"][0, s]), state_conv=f(inp["state_conv"][0, s]),
        )
        in_maps.append(m)
    used = set(nc._used_inputs)
    in_maps = [{k: v for k, v in m.items() if k in used} for m in in_maps]
    res = run_bass_kernel_spmd(nc, in_maps, core_ids=list(range(8)))
    R = res.results
    cat = lambda k: np.stack([np.asarray(R[c][k], np.float32) for c in range(8)])
    y_prompt = cat("y_prompt")
    y_sample = np.concatenate([R[c]["y_sample"] for c in range(8)], 0).reshape(128, 1, 1024)
    pool_p = cat("pool_p")[None]
    pool_s = np.concatenate([R[c]["pool_s"] for c in range(8)], 0)[None]
    shift_p = cat("shift_p").reshape(8, 1792)[None]
    shift_s = np.concatenate([R[c]["shift_s"] for c in range(8)], 0)[None]
    wkv_p = cat("wkv_p")[None]
    wkv_s = np.concatenate([R[c]["wkv_s"] for c in range(8)], 0)[None]
    conv_p = cat("conv_p")[None]
    conv_s = np.concatenate([R[c]["conv_s"] for c in range(8)], 0)[None]
    mem_k_p = np.stack([R[c]["mem_k_p"] for c in range(8)], 1).reshape(2, 8, 256, 4, 256)
    mem_v_p = np.stack([R[c]["mem_v_p"] for c in range(8)], 1).reshape(2, 8, 256, 4, 256)
    outs = (y_prompt, y_sample, pool_p, pool_s, shift_p, shift_s, wkv_p, wkv_s, conv_p, conv_s, mem_k_p, mem_v_p)
    return tuple(np.ascontiguousarray(o, dtype=np.float32) for o in outs)
```
